# Optimizing a Trainium2 kernel written in Bass

```python
import jax, jax.numpy as jnp
from jax import lax
import numpy as np

D_MODEL = 1024
BATCH = 16
SEQ = 2048
DEPTH = 1

PLE_DIM = 256
EPS = 1e-6
GM_WIDTH = 512
GM_GROUPS = 8
GM_CHUNK = 128
HG_HEADS = 4
HG_KEY = 128
HG_VAL = 128
HG_CHUNK = 64
HG_KW = HG_HEADS * HG_KEY
HG_VW = HG_HEADS * HG_VAL
IN_SPLITS = [GM_WIDTH, GM_WIDTH, HG_KW, HG_KW, HG_VW, HG_VW, D_MODEL, D_MODEL]
IN_COLS = sum(IN_SPLITS)
N_GROUPS = 4
EXP_PER_GROUP = 8
N_EXPERTS = N_GROUPS * EXP_PER_GROUP
TOP_K = 2
D_EXPERT = 512
MOE_BLOCK = 128

kernel_name = "hybrid_gmlp_hgrn2_hmoe_block"


def rmsnorm(x, g):
    xf = x.astype(jnp.float32)
    y = xf * lax.rsqrt(jnp.mean(xf * xf, axis=-1, keepdims=True) + EPS)
    return y.astype(x.dtype) * g


def layernorm(x, g, b):
    xf = x.astype(jnp.float32)
    mu = jnp.mean(xf, axis=-1, keepdims=True)
    var = jnp.mean(jnp.square(xf - mu), axis=-1, keepdims=True)
    y = (xf - mu) * lax.rsqrt(var + EPS)
    return y.astype(x.dtype) * g + b


def gmlp_branch(u, v, ln_g, ln_b, w_sp, b_sp):
    B, S, _ = u.shape
    n_c = S // GM_CHUNK
    dg = GM_WIDTH // GM_GROUPS
    v = layernorm(v, ln_g, ln_b).reshape(B, n_c, GM_CHUNK, GM_GROUPS, dg)
    mask = jnp.tril(jnp.ones((GM_CHUNK, GM_CHUNK), dtype=bool))
    w = jnp.where(mask[None], w_sp, jnp.zeros_like(w_sp))
    s = jnp.einsum('gts,bcsgd->bctgd', w, v) + b_sp.T[:, :, None]
    return u * s.reshape(B, S, GM_WIDTH)


def hgrn2_branch(q, f_raw, i_in, og, lb, norm_g):
    B, S, _ = q.shape
    C = HG_CHUNK
    n_c = S // C
    dt = q.dtype
    qf = jax.nn.silu(q.astype(jnp.float32)).reshape(B, n_c, C, HG_HEADS, HG_KEY)
    f = lb + (1.0 - lb) * jax.nn.sigmoid(f_raw.astype(jnp.float32))
    log_f = jnp.log(f).reshape(B, n_c, C, HG_HEADS, HG_KEY)
    k = (1.0 - f).reshape(B, n_c, C, HG_HEADS, HG_KEY)
    v = i_in.astype(jnp.float32).reshape(B, n_c, C, HG_HEADS, HG_VAL)
    b = jnp.cumsum(log_f, axis=2)
    b_last = b[:, :, -1:]
    q_dec = qf * jnp.exp(b)
    k_inv = k * jnp.exp(-b)
    k_end = k * jnp.exp(b_last - b)
    mask = jnp.tril(jnp.ones((C, C), dtype=bool))
    att = jnp.einsum('bcthk,bcshk->bchts', q_dec, k_inv)
    att = jnp.where(mask, att, jnp.zeros_like(att))
    o_intra = jnp.einsum('bchts,bcshv->bcthv', att, v)
    dS = jnp.einsum('bcshk,bcshv->bchkv', k_end, v)
    decay = jnp.exp(b_last[:, :, 0])

    def step(s_prev, inp):
        dec, ds = inp
        return dec[..., None] * s_prev + ds, s_prev

    s0 = jnp.zeros((B, HG_HEADS, HG_KEY, HG_VAL), jnp.float32)
    _, s_starts = lax.scan(step, s0, (jnp.moveaxis(decay, 1, 0), jnp.moveaxis(dS, 1, 0)))
    o_inter = jnp.einsum('bcthk,cbhkv->bcthv', q_dec, s_starts)
    o = (o_intra + o_inter).reshape(B, S, HG_HEADS, HG_VAL)
    o = o * lax.rsqrt(jnp.mean(o * o, axis=-1, keepdims=True) + EPS)
    o = o * norm_g.astype(jnp.float32).reshape(HG_HEADS, HG_VAL)
    return o.reshape(B, S, HG_VW).astype(dt) * jax.nn.sigmoid(og)


def hier_moe(h, w_grp, w_exp, w1, w3, w2):
    B, S, D = h.shape
    T = B * S
    xt = h.reshape(T, D)
    grp_logits = (xt @ w_grp).astype(jnp.float32)
    grp_prob = jax.nn.softmax(grp_logits, axis=-1)
    p_g, g_sel = lax.top_k(grp_prob, 1)
    exp_logits = (xt @ w_exp).astype(jnp.float32).reshape(T, N_GROUPS, EXP_PER_GROUP)
    sel_logits = jnp.take_along_axis(exp_logits, g_sel[:, :, None], axis=1)[:, 0]
    top_l, top_e = lax.top_k(sel_logits, TOP_K)
    gate = jax.nn.softmax(top_l, axis=-1) * p_g

    A = T * TOP_K
    eid = (g_sel * EXP_PER_GROUP + top_e).reshape(A)
    tok = jnp.repeat(jnp.arange(T, dtype=jnp.int32), TOP_K)
    wa = gate.reshape(A)
    order = jnp.argsort(eid)
    eid_s, tok_s, w_s = eid[order], tok[order], wa[order]
    counts = jnp.bincount(eid, length=N_EXPERTS)
    padded = ((counts + MOE_BLOCK - 1) // MOE_BLOCK) * MOE_BLOCK
    pad_end = jnp.cumsum(padded)
    pad_start = pad_end - padded
    start = jnp.cumsum(counts) - counts
    rank = jnp.arange(A, dtype=jnp.int32) - start[eid_s]
    dest = pad_start[eid_s] + rank
    n_blk = -(-A // MOE_BLOCK) + N_EXPERTS
    P = n_blk * MOE_BLOCK
    buf = jnp.zeros((P, D), xt.dtype).at[dest].set(xt[tok_s])
    blk_start = jnp.arange(n_blk, dtype=jnp.int32) * MOE_BLOCK
    blk_e = jnp.minimum(jnp.searchsorted(pad_end, blk_start, side='right'), N_EXPERTS - 1)

    def expert_block(args):
        xb, e = args
        return (jax.nn.silu(xb @ w1[e]) * (xb @ w3[e])) @ w2[e]

    ybuf = lax.map(expert_block, (buf.reshape(n_blk, MOE_BLOCK, D), blk_e)).reshape(P, D)
    contrib = ybuf[dest] * w_s[:, None].astype(xt.dtype)
    out = jax.ops.segment_sum(contrib, tok_s, num_segments=T)
    return out.reshape(B, S, D)


def setup_inputs(seed: int = 0) -> dict:
    key = jax.random.key(seed)
    ks = jax.random.split(key, 24)

    def nrm(k, shape, scale):
        return jax.random.normal(k, shape, jnp.float32) * scale

    def gain(k, shape):
        return 1.0 + 0.05 * jax.random.normal(k, shape, jnp.float32)

    L = DEPTH
    return {
        "x": nrm(ks[0], (BATCH, SEQ, D_MODEL), 1.0),
        "p": nrm(ks[1], (DEPTH, BATCH, SEQ, PLE_DIM), 1.0),
        "g_mix": gain(ks[2], (L, D_MODEL)),
        "w_in": nrm(ks[3], (L, D_MODEL, IN_COLS), D_MODEL ** -0.5),
        "gm_ln_g": gain(ks[4], (L, GM_WIDTH)),
        "gm_ln_b": nrm(ks[5], (L, GM_WIDTH), 0.02),
        "gm_w_sp": nrm(ks[6], (L, GM_GROUPS, GM_CHUNK, GM_CHUNK), 0.1 * GM_CHUNK ** -0.5),
        "gm_b_sp": gain(ks[7], (L, GM_GROUPS, GM_CHUNK)),
        "w_up_a": nrm(ks[8], (L, GM_WIDTH, D_MODEL), GM_WIDTH ** -0.5),
        "hg_lb_param": gain(ks[9], (L + 1, HG_KW)),
        "hg_norm_g": gain(ks[10], (L, HG_VW)),
        "w_up_b": nrm(ks[11], (L, HG_VW, D_MODEL), HG_VW ** -0.5),
        "w_out": nrm(ks[12], (L, D_MODEL, D_MODEL), D_MODEL ** -0.5),
        "g_ffn": gain(ks[13], (L, D_MODEL)),
        "w_grp": nrm(ks[14], (L, D_MODEL, N_GROUPS), D_MODEL ** -0.5),
        "w_exp": nrm(ks[15], (L, D_MODEL, N_EXPERTS), D_MODEL ** -0.5),
        "w1": nrm(ks[16], (L, N_EXPERTS, D_MODEL, D_EXPERT), D_MODEL ** -0.5),
        "w3": nrm(ks[17], (L, N_EXPERTS, D_MODEL, D_EXPERT), D_MODEL ** -0.5),
        "w2": nrm(ks[18], (L, N_EXPERTS, D_EXPERT, D_MODEL), D_EXPERT ** -0.5),
        "g_ple": gain(ks[19], (L, D_MODEL)),
        "w_pg": nrm(ks[20], (L, D_MODEL, D_MODEL), D_MODEL ** -0.5),
        "w_ple": nrm(ks[21], (L, PLE_DIM, D_MODEL), PLE_DIM ** -0.5),
        "g_final": gain(ks[22], (D_MODEL,)),
    }


def reference(x, p, g_mix, w_in, gm_ln_g, gm_ln_b, gm_w_sp, gm_b_sp, w_up_a,
              hg_lb_param, hg_norm_g, w_up_b, w_out, g_ffn, w_grp, w_exp,
              w1, w3, w2, g_ple, w_pg, w_ple, g_final):
    lb_all = jnp.cumsum(jax.nn.softmax(hg_lb_param.astype(jnp.float32), axis=0), axis=0)
    split_idx = [int(s) for s in np.cumsum(IN_SPLITS)[:-1]]
    for i in range(DEPTH):
        h = rmsnorm(x, g_mix[i])
        z = h @ w_in[i]
        u, v, hq, hf, hi, hog, gate_a, gate_b = jnp.split(z, split_idx, axis=-1)
        y_a = gmlp_branch(jax.nn.gelu(u), jax.nn.gelu(v), gm_ln_g[i], gm_ln_b[i], gm_w_sp[i], gm_b_sp[i])
        y_b = hgrn2_branch(hq, hf, hi, hog, lb_all[i], hg_norm_g[i])
        merged = jax.nn.sigmoid(gate_a) * (y_a @ w_up_a[i]) + jax.nn.sigmoid(gate_b) * (y_b @ w_up_b[i])
        x = x + merged @ w_out[i]
        h = rmsnorm(x, g_ffn[i])
        x = x + hier_moe(h, w_grp[i], w_exp[i], w1[i], w3[i], w2[i])
        h = rmsnorm(x, g_ple[i])
        x = x + jax.nn.sigmoid(h @ w_pg[i]) * (p[i] @ w_ple[i])
    return rmsnorm(x, g_final)
```

```python
import contextlib
import numpy as np
import concourse.bass as bass
import concourse.mybir as mybir
from concourse.bass_utils import run_bass_kernel_spmd

F32 = mybir.dt.float32
BF16 = mybir.dt.bfloat16
I32 = mybir.dt.int32
ALU = mybir.AluOpType
AF = mybir.ActivationFunctionType
AX = mybir.AxisListType

import os
MODE = 2
KV = int(os.environ.get('KV', '1'))
NCORES = 8
TOK = 4096
NT = TOK // 128
TPS = 16
D = 1024
NBLK = 64
BR = 256
SKIP0 = 51
EPS = 1e-6
EPOCH = 4000
ENGS = ("pe", "act", "dve", "pool", "sp")


class Op:
    __slots__ = ("eng", "fn", "deps", "has_dep", "dma", "sem", "val", "idx", "force", "n")

    def __init__(self, eng, fn, dma, idx):
        self.eng = eng
        self.fn = fn
        self.dma = dma
        self.deps = set()
        self.has_dep = False
        self.sem = None
        self.val = 0
        self.idx = idx
        self.force = False
        self.n = 1


class Sched:
    def __init__(self):
        self.ops = []
        self.last_w = {}
        self.readers = {}

    def add(self, eng, fn, reads=(), writes=(), dma=None, after=(), n=1):
        op = Op(eng, fn, dma, len(self.ops))
        op.n = n
        deps = set(a for a in after if a is not None)
        for k in reads:
            w = self.last_w.get(k)
            if w is not None:
                deps.add(w)
        for k in writes:
            w = self.last_w.get(k)
            if w is not None:
                deps.add(w)
            for r in self.readers.get(k, ()):
                deps.add(r)
        for k in reads:
            self.readers.setdefault(k, []).append(op)
        for k in writes:
            self.last_w[k] = op
            self.readers[k] = []
        deps.discard(op)
        if eng == "pe":
            deps = set(d for d in deps if d.eng != "pe" or d.dma is not None)
        op.deps = deps
        for d in deps:
            d.has_dep = True
        self.ops.append(op)
        return op

    def emit(self, nc, final_wait_eng="sp"):
        cnt = {e: 0 for e in ENGS}
        dma_cnt = {}
        sem_names = set()
        for op in self.ops:
            if op.dma is not None:
                dma_cnt[op.dma] = dma_cnt.get(op.dma, 0) + op.n
                op.sem = "dma_" + op.dma
                op.val = 16 * dma_cnt[op.dma]
                sem_names.add(op.sem)
            elif op.has_dep:
                c = cnt[op.eng]
                cnt[op.eng] = c + 1
                op.sem = "c_%s_%d" % (op.eng, c // EPOCH)
                op.val = (c % EPOCH) + 1
                sem_names.add(op.sem)
        sem_names = sorted(sem_names)
        self.n_sems = len(sem_names)
        with contextlib.ExitStack() as st:
            sems = {n: st.enter_context(nc.semaphore(n)) for n in sem_names}
            block = st.enter_context(nc.Block())
            final = {}
            for op in self.ops:
                if op.dma is not None:
                    final[op.sem] = max(final.get(op.sem, 0), op.val)

            def run(engname, eng):
                waited = {}
                for op in self.ops:
                    if op.eng != engname:
                        continue
                    need = {}
                    for d in op.deps:
                        if waited.get(d.sem, 0) < d.val:
                            need[d.sem] = max(need.get(d.sem, 0), d.val)
                    for s, v in need.items():
                        eng.wait_ge(sems[s], v)
                        waited[s] = v
                    inst = op.fn(eng)
                    if op.sem is not None:
                        insts = inst if isinstance(inst, (list, tuple)) else [inst]
                        for ii in insts:
                            ii.then_inc(sems[op.sem], 16 if op.dma is not None else 1)
                if engname == final_wait_eng:
                    for s, v in final.items():
                        if waited.get(s, 0) < v:
                            eng.wait_ge(sems[s], v)

            @block.tensor
            def _(e):
                run("pe", e)

            @block.scalar
            def _(e):
                run("act", e)

            @block.vector
            def _(e):
                run("dve", e)

            @block.gpsimd
            def _(e):
                run("pool", e)

            @block.sync
            def _(e):
                run("sp", e)


def build(dbg=False, nt=NT, nblk=NBLK, phases="ABCD"):
    nc = bass.Bass("TRN2", target_bir_lowering=False)
    S = Sched()
    st = contextlib.ExitStack()

    def din(name, shape, dt=F32):
        return nc.dram_tensor(name, list(shape), dt, kind="ExternalInput").ap()

    x_d = din("x", [TOK, D])
    p_d = din("p", [TOK, 256])
    g_mix_d = din("g_mix", [1, D])
    w_in_d = din("w_in", [1, D, 5120])
    ln_g_d = din("gm_ln_g", [1, 512])
    ln_b_d = din("gm_ln_b", [1, 512])
    w_sp_d = din("gm_w_sp", [1, 8, 128, 128])
    b_sp_d = din("gm_b_sp", [1, 8, 128])
    w_up_a_d = din("w_up_a", [1, 512, D])
    lbp_d = din("hg_lb_param", [2, 512])
    ng_d = din("hg_norm_g", [1, 512])
    w_up_b_d = din("w_up_b", [1, 512, D])
    w_out_d = din("w_out", [1, D, D])
    g_ffn_d = din("g_ffn", [1, D])
    w_grp_d = din("w_grp", [1, D, 4])
    w_exp_d = din("w_exp", [1, D, 32])
    w1_d = din("w1", [1, 32, D, 512])
    w3_d = din("w3", [1, 32, D, 512])
    w2_d = din("w2", [1, 32, 512, D])
    g_ple_d = din("g_ple", [1, D])
    w_pg_d = din("w_pg", [1, D, D])
    w_ple_d = din("w_ple", [1, 256, D])
    g_final_d = din("g_final", [1, D])
    out_d = nc.dram_tensor("out", [TOK, D], F32, kind="ExternalOutput").ap()
    x1_d = nc.dram_tensor("x1_scr", [TOK, D], F32, kind="Internal").ap()
    h2_d = nc.dram_tensor("h2_scr", [TOK, D], BF16, kind="Internal").ap()
    xbuf_d = nc.dram_tensor("xbuf_scr", [NBLK * BR, D], BF16, kind="Internal").ap()
    ybuf_d = nc.dram_tensor("ybuf_scr", [NBLK * BR, D], BF16, kind="Internal").ap()
    meta_d = nc.dram_tensor("meta_scr", [128, 2], I32, kind="Internal").ap()
    zsrc_d = nc.dram_tensor("zsrc_scr", [512, D], BF16, kind="Internal").ap()
    dbg_d = {}
    if dbg:
        dbg_d["x1"] = nc.dram_tensor("dbg_x1", [TOK, D], F32, kind="ExternalOutput").ap()
        dbg_d["meta"] = nc.dram_tensor("dbg_meta", [128, 2], I32, kind="ExternalOutput").ap()
        dbg_d["dest"] = nc.dram_tensor("dbg_dest", [128, 64], I32, kind="ExternalOutput").ap()
        dbg_d["gate"] = nc.dram_tensor("dbg_gate", [128, 64], F32, kind="ExternalOutput").ap()
        dbg_d["x2"] = nc.dram_tensor("dbg_x2", [TOK, D], F32, kind="ExternalOutput").ap()

    def sb(name, shape, dt):
        return st.enter_context(nc.sbuf_tensor(name, list(shape), dt))

    def ps(name, shape, dt):
        return st.enter_context(nc.psum_tensor(name, list(shape), dt))

    wbig = sb("wbig", [128, 40960], BF16)
    wua = sb("wua", [128, 4, D], BF16)
    wub = sb("wub", [128, 4, D], BF16)
    wo = sb("wo", [128, 8, D], BF16)
    ident_b = sb("ident_b", [128, 128], BF16)
    ident_f = sb("ident_f", [128, 128], F32)
    ones_f = sb("ones_f", [128, 128], F32)
    ones_b = sb("ones_b", [128, 128], BF16)
    stri_b = sb("stri_b", [128, 128], BF16)
    tri_f = sb("tri_f", [128, 128], F32)
    hmask4 = sb("hmask4", [128, 512], BF16)
    wspT = sb("wspT", [128, 8, 128], BF16)
    bsp = sb("bsp", [128, 8], F32)
    lng_bc = sb("lng_bc", [128, 512], BF16)
    lnb_bc = sb("lnb_bc", [128, 512], BF16)
    ng_bc = sb("ng_bc", [128, 512], BF16)
    gbc0 = sb("gbc0", [128, D], BF16)
    gbc1 = sb("gbc1", [128, D], BF16)
    wr = sb("wr", [128, 8, 36], BF16)
    rmask = sb("rmask", [128, 512], BF16)
    pmask = sb("pmask", [128, 1], F32)
    lbp = sb("lbp", [128, 2, 4], F32)
    lb = sb("lb", [128, 4], F32)
    oml = sb("oml", [128, 4], F32)
    nhalf = sb("nhalf", [128, 4], F32)
    bstart = sb("bstart", [128, 1], F32)
    Jt = sb("Jt", [128, 96], F32)
    pid = sb("pid", [128, 1], F32)
    blkrow = sb("blkrow", [128, 96], F32)
    idxall = sb("idxall", [128, 96], I32)

    xt = [sb("xt0", [128, D], F32), sb("xt1", [128, D], F32)]
    bigf2 = sb("bigf2", [128, D], F32)
    bigf3 = sb("bigf3", [128, D], F32)
    B8 = sb("B8", [128, D], BF16)
    B8b = sb("B8b", [128, D], BF16)
    T8 = sb("T8", [128, 8, 128], BF16)
    T8f = sb("T8f", [128, 8, 128], BF16)
    T4 = sb("T4", [128, 4, 128], BF16)
    tmpA = sb("tmpA", [128, 512], F32)
    gu = [sb("gu0", [128, 512], BF16), sb("gu1", [128, 512], BF16)]
    gv = bigf3[:, 512:1024]
    vn = [sb("vn0", [128, 512], BF16), sb("vn1", [128, 512], BF16)]
    ya = sb("ya", [128, 512], BF16)
    v_tok = [sb("v_tok0", [128, 512], BF16), sb("v_tok1", [128, 512], BF16)]
    sog = [sb("sog0", [128, 512], BF16), sb("sog1", [128, 512], BF16)]
    sga = [sb("sga0", [128, D], BF16), sb("sga1", [128, D], BF16)]
    sgb = [sb("sgb0", [128, D], BF16), sb("sgb1", [128, D], BF16)]
    F1 = [sb("F1_0", [128, 512], F32)]
    F2 = sb("F2", [128, 512], F32)
    F3 = sb("F3", [128, 512], F32)
    F4 = sb("F4", [128, 512], F32)
    F5 = sb("F5", [128, 512], F32)
    F6 = [sb("F6_0", [128, 512], BF16), sb("F6_1", [128, 512], BF16)]
    _al = bigf3[:, 0:512].bitcast(BF16)
    qdT = [sb("qdT0", [128, 512], BF16), _al[:, 0:512]]
    kiT = [sb("kiT0", [128, 512], BF16), _al[:, 512:1024]]
    keT = [sb("keT0", [128, 512], BF16), F6[1]]
    dec = sb("dec", [128, 16], F32)
    ke_tok = sb("ke_tok", [128, 512], BF16)
    attT = sb("attT", [128, 512], BF16)
    Sst = sb("Sst", [128, 512], F32)
    Sbf = sb("Sbf", [128, 512], BF16)
    yb = sb("yb", [128, 512], BF16)
    m2h = sb("m2h", [128, 512], F32)
    tmpB = m2h
    st6 = sb("st6", [128, 6], F32)
    sm = sb("sm", [128, 32], F32)
    lg = sb("lg", [128, 36], F32)
    rt = sb("rt", [128, 64], F32)
    ridx = sb("ridx", [128, 16], mybir.dt.uint32)
    io8 = sb("io8", [128, 8], F32)
    M1a = sb("M1a", [128, NT, 32], BF16)
    M2a = sb("M2a", [128, NT, 32], BF16)
    Msum = [sb("Msum0", [128, 32], BF16), sb("Msum1", [128, 32], BF16)]
    rka = sb("rka", [128, NT, 32], F32)
    gla = sb("gla", [128, NT, 4], F32)
    gmxa = sb("gmxa", [128, NT], F32)
    d12a = sb("d12a", [128, NT], F32)
    g1a = sb("g1a", [128, NT], F32)
    g2a = sb("g2a", [128, NT], F32)
    runtot = sb("runtot", [128, 32], F32)
    pb = sb("pb", [128, 6, 32], F32)
    pbi = sb("pbi", [128, 32], I32)
    d1f = sb("d1f", [128, NT], F32)
    d2f = sb("d2f", [128, NT], F32)
    dsti = sb("dsti", [128, 2 * NT], I32)
    metaf = sb("metaf", [128, 2], F32)
    metai = sb("metai", [128, 2], I32)
    ptile = F3[:, 0:256]
    ptb = sb("ptb", [128, 256], BF16)

    pT = ps("pT", [128, 1024], BF16)
    pQ = ps("pQ", [128, 512], F32)
    pF = ps("pF", [128, 512], F32)
    pM = [ps("pM0", [128, 512], F32), ps("pM1", [128, 512], F32)]
    pO = ps("pO", [128, 512], F32)
    pD = ps("pD", [128, 512], F32)
    pA = ps("pA", [128, 512], F32)
    mrot = [0]

    def nextM():
        i = mrot[0] % 2
        mrot[0] += 1
        return pM[i], "pM%d" % i

    A = S.add

    def mm(out, lhsT, rhs, start, stop, reads, writes, **kw):
        return A("pe", lambda e: e.matmul(out=out, lhsT=lhsT, rhs=rhs, start=start, stop=stop, **kw), reads=reads, writes=writes)

    def tr(out, in_, ident, reads, writes):
        return A("pe", lambda e: e.transpose(out=out, in_=in_, identity=ident), reads=reads, writes=writes)

    def act(out, in_, func, reads, writes, **kw):
        return A("act", lambda e: e.activation(out=out, in_=in_, func=func, **kw), reads=reads, writes=writes)

    def tt(eng, out, in0, in1, op, reads, writes):
        return A(eng, lambda e: e.tensor_tensor(out=out, in0=in0, in1=in1, op=op), reads=reads, writes=writes)

    def ts(eng, out, in0, s1, s2, op0, op1, reads, writes):
        if s2 is None:
            return A(eng, lambda e: e.tensor_scalar(out=out, in0=in0, scalar1=s1, scalar2=None, op0=op0), reads=reads, writes=writes)
        return A(eng, lambda e: e.tensor_scalar(out=out, in0=in0, scalar1=s1, scalar2=s2, op0=op0, op1=op1), reads=reads, writes=writes)

    def stt(out, in0, scalar, in1, op0, op1, reads, writes, accum=None):
        if accum is None:
            return A("dve", lambda e: e.scalar_tensor_tensor(out=out, in0=in0, scalar=scalar, in1=in1, op0=op0, op1=op1), reads=reads, writes=writes)
        return A("dve", lambda e: e.scalar_tensor_tensor(out=out, in0=in0, scalar=scalar, in1=in1, op0=op0, op1=op1, accum_out=accum), reads=reads, writes=writes)

    def cp(eng, out, in_, reads, writes):
        if eng == "act":
            return A("act", lambda e: e.copy(out=out, in_=in_), reads=reads, writes=writes)
        return A(eng, lambda e: e.tensor_copy(out=out, in_=in_), reads=reads, writes=writes)

    def dma(q, out, in_, key, reads, writes, **kw):
        return A(q, lambda e: e.dma_start(out=out, in_=in_, **kw), reads=reads, writes=writes, dma=key)

    def memset(eng, ap, val, writes):
        return A(eng, lambda e: e.memset(ap, val), writes=writes)

    with st:
        for kc in range(8):
            dma("pool", wbig[:, kc * 5120:(kc + 1) * 5120], w_in_d[0, kc * 128:(kc + 1) * 128, :], "win%d" % kc, [], ["win%d" % kc])
        memset("pool", ones_f[:], 1.0, ["ones_f"])
        memset("pool", ones_b[:], 1.0, ["ones_b"])
        memset("pool", nhalf[:], -0.5, ["nhalf"])
        A("pool", lambda e: e.affine_select(out=ident_f[:], in_=ones_f[:], pattern=[[1, 128]], compare_op=ALU.is_equal,
                                            fill=0.0, base=0, channel_multiplier=-1), reads=["ones_f"], writes=["ident_f"])
        A("pool", lambda e: e.affine_select(out=ident_b[:], in_=ones_f[:], pattern=[[1, 128]], compare_op=ALU.is_equal,
                                            fill=0.0, base=0, channel_multiplier=-1), reads=["ones_f"], writes=["ident_b"])
        A("pool", lambda e: e.affine_select(out=tri_f[:], in_=ones_f[:], pattern=[[1, 128]], compare_op=ALU.is_ge,
                                            fill=0.0, base=0, channel_multiplier=-1), reads=["ones_f"], writes=["tri_f"])
        A("pool", lambda e: e.affine_select(out=stri_b[:], in_=ones_f[:], pattern=[[1, 128]], compare_op=ALU.is_gt,
                                            fill=0.0, base=0, channel_multiplier=-1), reads=["ones_f"], writes=["stri_b"])
        for h in range(4):
            cp("pool", hmask4[:, h * 128:(h + 1) * 128], tri_f[:], ["tri_f"], ["hmask4"])
        for h in range(4):
            memset("pool", hmask4[0:64, h * 128 + 64:(h + 1) * 128], 0.0, ["hmask4"])
        memset("pool", rmask[:], 1.0, ["rmask"])
        for c in range(0, 512, 64):
            memset("pool", rmask[:, c:c + 1], 0.0, ["rmask"])
        A("pool", lambda e: e.iota(out=bstart[:], pattern=[[0, 1]], base=0, channel_multiplier=BR,
                                   allow_small_or_imprecise_dtypes=True), writes=["bstart"])
        memset("pool", runtot[:], 0.0, ["runtot"])
        memset("pool", pmask[:], 1.0, ["pmask"])
        memset("pool", blkrow[:], 0.0, ["blkrow"])
        memset("pool", rt[:], 0.0, ["rt"])
        memset("pool", rt[:, 44:48], -1.0e30, ["rt"])
        memset("pool", ridx[:], 0, ["ridx"])
        A("pool", lambda e: e.iota(out=io8[:], pattern=[[1, 8]], base=0, channel_multiplier=0,
                                   allow_small_or_imprecise_dtypes=True), writes=["io8"])
        memset("pool", sm[:], 0.0, ["sm0", "sm1", "sm2", "sm3", "sm4", "sm6", "sm8"])
        memset("pool", pb[:], 0.0, ["pb0", "pb1", "pb2", "pb3", "pb4", "pb5"])
        memset("pool", dec[:], 0.0, ["dec0", "dec1"])
        memset("pool", pmask[0:1, :], 0.0, ["pmask"])
        A("pool", lambda e: e.iota(out=pid[:], pattern=[[0, 1]], base=0, channel_multiplier=1,
                                   allow_small_or_imprecise_dtypes=True), writes=["pid"])
        A("pool", lambda e: e.iota(out=Jt[:], pattern=[[BR, 96]], base=0, channel_multiplier=0,
                                   allow_small_or_imprecise_dtypes=True), writes=["Jt"])

        memset("pool", bigf2[:], 0.0, ["bigf2"])
        zsrc = bigf2[:].bitcast(BF16).rearrange("p (r d) -> p r d", r=2)
        zero_ops = []
        for zi in range(2):
            dma("sp", zsrc_d[zi * 256:(zi + 1) * 256, :].rearrange("(r p) d -> p r d", p=128), zsrc, "zsrc", ["bigf2"], ["zsrc_d"])
        WIN = ["win%d" % k for k in range(8)]
        dma("pool", wua[:], w_up_a_d[0].rearrange("(c p) n -> p c n", p=128), "wua", [], ["wua"])
        dma("pool", wub[:], w_up_b_d[0].rearrange("(c p) n -> p c n", p=128), "wub", [], ["wub"])
        dma("pool", wo[:], w_out_d[0].rearrange("(c p) n -> p c n", p=128), "wo", [], ["wo"])
        dma("pool", wr[:, :, 0:4], w_grp_d[0].rearrange("(c p) n -> p c n", p=128), "wr0", [], ["wr0"])
        dma("pool", wr[:, :, 4:36], w_exp_d[0].rearrange("(c p) n -> p c n", p=128), "wr1", [], ["wr1"])
        dma("pool", gbc0[:], g_mix_d.partition_broadcast(128), "gbc0", [], ["gbc0"])
        dma("pool", lng_bc[:], ln_g_d.partition_broadcast(128), "lng", [], ["lng_bc"])
        dma("pool", lnb_bc[:], ln_b_d.partition_broadcast(128), "lnb", [], ["lnb_bc"])
        dma("pool", ng_bc[:], ng_d.partition_broadcast(128), "ngb", [], ["ng_bc"])
        dma("pool", gbc1[:], g_ffn_d.partition_broadcast(128), "gbc1", [], ["gbc1"])
        dma("sp", bsp[:], b_sp_d[0].rearrange("g t -> t g"), "bsp", [], ["bsp"], allow_slow_non_contiguous=True)
        dma("sp", lbp[:], lbp_d.rearrange("s (h k) -> k s h", k=128), "lbp", [], ["lbp"], allow_slow_non_contiguous=True)
        tt("dve", lb[:], lbp[:, 0, :], lbp[:, 1, :], ALU.subtract, ["lbp"], ["lb"])
        act(lb[:], lb[:], AF.Sigmoid, ["lb"], ["lb"])
        ts("dve", oml[:], lb[:], -1.0, 1.0, ALU.mult, ALU.add, ["lb"], ["oml"])
        wraw = bigf2
        dma("sp", wraw[:].rearrange("p (g s) -> p g s", g=8), w_sp_d[0].rearrange("g t s -> t g s"), "wraw", [], ["bigf2"])
        for half in range(2):
            for j in range(4):
                g = half * 4 + j
                tr(pQ[:, j * 128:(j + 1) * 128], wraw[:, g * 128:(g + 1) * 128], ident_f[:], ["bigf2", "ident_f"], ["pQ"])
            for j in range(4):
                g = half * 4 + j
                tt("dve", wspT[:, g, :], pQ[:, j * 128:(j + 1) * 128], tri_f[:], ALU.mult, ["pQ", "tri_f"], ["wspT"])

        def rmsnorm_to_bf16(src, src_key, gbc, gkey, dst, dst_key, smcol):
            act(dst[:], src[:], AF.Square, [src_key], [dst_key, "sm%d" % smcol], accum_out=sm[:, smcol:smcol + 1])
            ts("pool", sm[:, smcol:smcol + 1], sm[:, smcol:smcol + 1], 1.0 / D, EPS, ALU.mult, ALU.add, ["sm%d" % smcol], ["sm%d" % smcol])
            tt("pool", sm[:, smcol:smcol + 1], sm[:, smcol:smcol + 1], nhalf[:, 0:1], ALU.pow, ["sm%d" % smcol, "nhalf"], ["sm%d" % smcol])
            stt(dst[:], src[:], sm[:, smcol:smcol + 1], gbc[:], ALU.mult, ALU.mult, [src_key, "sm%d" % smcol, gkey], [dst_key])

        def transpose8(src, src_key, dst, dst_key, n=8, stride=None):
            for kc in range(n):
                if stride is None:
                    sl = src[:, kc * 128:(kc + 1) * 128]
                else:
                    sl = src[:, kc:128 * stride:stride]
                tr(pT[:, kc * 128:(kc + 1) * 128], sl, ident_b[:], [src_key, "ident_b"], ["pT"])
            cp("act", dst[:, 0:n, :].rearrange("p c t -> p (c t)") if False else dst[:].rearrange("p c t -> p (c t)"), pT[:, 0:n * 128], ["pT"], [dst_key])

        def gelu_from_psum(pbank, pkey, dst, dst_key):
            act(tmpA[:], pbank[:], AF.Square, [pkey], ["tmpA"], scale=0.21145921592579985)
            stt(tmpA[:], tmpA[:], 1.0, pbank[:], ALU.add, ALU.mult, ["tmpA", pkey], ["tmpA"])
            act(tmpA[:], tmpA[:], AF.Sigmoid, ["tmpA"], ["tmpA"], scale=1.5957691216057308)
            tt("dve", dst[:], tmpA[:], pbank[:], ALU.mult, ["tmpA", pkey], [dst_key])

        last_inproj = [None]

        def front(t):
            par = t % 2
            xb_ = xt[par]
            xk = "xt%d" % par
            dma("sp", xb_[:], x_d[t * 128:(t + 1) * 128, :], xk, [], [xk])
            yield
            rmsnorm_to_bf16(xb_, xk, gbc0, "gbc0", B8b, "B8b", 0)
            yield
            yield
            transpose8(B8b, "B8b", T8f, "T8f")
            hT = T8f
            yield
            for (pb_, pk, c0) in ((pQ, "pQ", 1024), (pF, "pF", 1536)):
                for h in range(4):
                    for kc in range(8):
                        last_inproj[0] = mm(pb_[:, h * 128:(h + 1) * 128], wbig[:, kc * 5120 + c0 + h * 128: kc * 5120 + c0 + (h + 1) * 128],
                                            hT[:, kc, :], kc == 0, kc == 7, ["T8f"] + WIN, [pk])
                if pk == "pQ":
                    act(F6[0][:], pQ[:], AF.Sigmoid, ["pQ"], ["F6_0"])
                    tt("dve", F6[0][:], F6[0][:], pQ[:], ALU.mult, ["F6_0", "pQ"], ["F6_0"])
                else:
                    act(F1[0][:], pF[:], AF.Sigmoid, ["pF"], ["F1_0"])
                yield

            def inproj_tok(c0):
                bank, bk = nextM()
                for kc in range(8):
                    last_inproj[0] = mm(bank[:], hT[:, kc, :], wbig[:, kc * 5120 + c0: kc * 5120 + c0 + 512], kc == 0, kc == 7, ["T8f"] + WIN, [bk])
                return bank, bk

            bv, bvk = inproj_tok(512)
            gelu_from_psum(bv, bvk, gv, "gv")
            yield
            F1p, F1k = F1[0], "F1_0"
            for h in range(4):
                ts("dve", F1p[:, h * 128:(h + 1) * 128], F1p[:, h * 128:(h + 1) * 128], oml[:, h:h + 1], lb[:, h:h + 1], ALU.mult, ALU.add,
                   [F1k, "oml", "lb"], [F1k])
            ts("pool", F2[:], F1p[:], -1.0, 1.0, ALU.mult, ALU.add, [F1k], ["F2"])
            tt("pool", F3[:], F1p[:], rmask[:], ALU.mult, [F1k, "rmask"], ["F3"])
            tt("pool", F4[:], F1p[:], F3[:], ALU.subtract, [F1k, "F3"], ["F4"])
            yield
            A("dve", lambda e: e.bn_stats(out=st6[:], in_=gv[:]), reads=["gv"], writes=["st6"])
            A("dve", lambda e: e.bn_aggr(out=sm[:, 4:6], in_=st6[:]), reads=["st6"], writes=["sm4"])
            ts("pool", sm[:, 6:7], sm[:, 5:6], EPS, None, ALU.add, None, ["sm4"], ["sm6"])
            tt("pool", sm[:, 6:7], sm[:, 6:7], nhalf[:, 0:1], ALU.pow, ["sm6", "nhalf"], ["sm6"])
            ts("dve", gv[:], gv[:], sm[:, 4:5], sm[:, 6:7], ALU.subtract, ALU.mult, ["gv", "sm4", "sm6"], ["gv"])
            tt("pool", gv[:], gv[:], lng_bc[:], ALU.mult, ["gv", "lng_bc"], ["gv"])
            tt("pool", vn[par][:], gv[:], lnb_bc[:], ALU.add, ["gv", "lnb_bc"], ["vn%d" % par])
            yield
            bu, buk = inproj_tok(0)
            gelu_from_psum(bu, buk, gu[par], "gu%d" % par)
            yield
            A("dve", lambda e: e.tensor_tensor_scan(out=F5[:], data0=F3[:], data1=F4[:], initial=0.0, op0=ALU.mult, op1=ALU.add),
              reads=["F3", "F4"], writes=["F5"])
            A("dve", lambda e: e.reciprocal(out=F3[:], in_=F5[:]), reads=["F5"], writes=["F3"])
            tt("dve", qdT[par][:], F6[0][:], F5[:], ALU.mult, ["F6_0", "F5"], ["qdT%d" % par])
            tt("pool", kiT[par][:], F2[:], F3[:], ALU.mult, ["F2", "F3"], ["kiT%d" % par])
            yield
            bi, bik = inproj_tok(2048)
            cp("act", v_tok[par][:], bi[:], [bik], ["v_tok%d" % par])
            yield
            for h in range(4):
                for c in range(2):
                    lo = h * 128 + c * 64
                    stt(keT[par][:, lo:lo + 64], F2[:, lo:lo + 64], F5[:, lo + 63:lo + 64], F3[:, lo:lo + 64], ALU.mult, ALU.mult,
                        ["F2", "F5", "F3"], ["keT%d" % par])
            cp("pool", dec[:, par * 8:(par + 1) * 8], F5[:, 63:512:64], ["F5"], ["dec%d" % par])
            yield
            bo, bok = inproj_tok(2560)
            act(sog[par][:], bo[:], AF.Sigmoid, [bok], ["sog%d" % par])
            yield
            for j in range(2):
                bg, bgk = inproj_tok(3072 + j * 512)
                act(sga[par][:, j * 512:(j + 1) * 512], bg[:], AF.Sigmoid, [bgk], ["sga%d" % par])
                yield
            for j in range(2):
                bg, bgk = inproj_tok(4096 + j * 512)
                act(sgb[par][:, j * 512:(j + 1) * 512], bg[:], AF.Sigmoid, [bgk], ["sgb%d" % par])
                yield

        gdone = {}

        def back_g(t):
            par = t % 2
            bs_, bsk = nextM()
            for g in range(8):
                gs = slice(g * 64, (g + 1) * 64)
                mm(bs_[:, gs], wspT[:, g, :], vn[par][:, gs], True, True, ["wspT", "vn%d" % par], [bsk])
            for g in range(8):
                gs = slice(g * 64, (g + 1) * 64)
                stt(ya[:, gs], bs_[:, gs], bsp[:, g:g + 1], gu[par][:, gs], ALU.add, ALU.mult, [bsk, "bsp", "gu%d" % par], ["ya"])
            yield
            T4g = T8[:, 4:8, :]
            transpose8(ya, "ya", T4g, "T8", n=4)
            yield
            banksA = []
            for half in range(2):
                bk_, bkk = nextM()
                for c in range(4):
                    mm(bk_[:], T4g[:, c, :], wua[:, c, half * 512:(half + 1) * 512], c == 0, c == 3, ["T8", "wua"], [bkk])
                banksA.append((bk_, bkk))
            for half in range(2):
                bk_, bkk = banksA[half]
                tt("dve", bigf2[:, half * 512:(half + 1) * 512], bk_[:], sga[par][:, half * 512:(half + 1) * 512], ALU.mult, [bkk, "sga%d" % par], ["bigf2"])
            yield
            gdone[t] = True
            yield

        def back(t):
            par = t % 2
            xb_ = xt[par]
            xk = "xt%d" % par
            qd, qdk = qdT[par], "qdT%d" % par
            ki, kik = kiT[par], "kiT%d" % par
            ke, kek = keT[par], "keT%d" % par
            vtk, vtkk = v_tok[par], "v_tok%d" % par
            if t % TPS == 0:
                memset("pool", Sst[:], 0.0, ["Sst"])
                memset("pool", Sbf[:], 0.0, ["Sbf"])
            for h in range(4):
                tr(pT[:, h * 128:(h + 1) * 128], ke[:, h * 128:(h + 1) * 128], ident_b[:], [kek, "ident_b"], ["pT"])
            cp("act", ke_tok[:], pT[:, 0:512], ["pT"], ["ke_tok"])
            for h in range(4):
                hs = slice(h * 128, (h + 1) * 128)
                mm(pA[:, hs], ki[:, hs], qd[:, hs], True, True, [kik, qdk], ["pA"])
            tt("dve", attT[:], pA[:], hmask4[:], ALU.mult, ["pA", "hmask4"], ["attT"])
            yield
            if t >= 1:
                rank_step(t - 1)
            for h in range(4):
                hs = slice(h * 128, (h + 1) * 128)
                mm(pD[:, hs], ke_tok[0:64, hs], vtk[0:64, hs], True, True, ["ke_tok", vtkk], ["pD"])
            for h in range(4):
                hs = slice(h * 128, (h + 1) * 128)
                mm(pO[:, hs], attT[:, hs], vtk[:, hs], h == 0, False, ["attT", vtkk], ["pO"], skip_group_check=True)
                mm(pO[0:64, hs], qd[:, h * 128:h * 128 + 64], Sbf[:, hs], False, False, [qdk, "Sbf"], ["pO"], skip_group_check=True)
            for h in range(4):
                hs = slice(h * 128, (h + 1) * 128)
                stt(Sst[:, hs], Sst[:, hs], dec[:, par * 8 + 2 * h:par * 8 + 2 * h + 1], pD[:, hs], ALU.mult, ALU.add, ["Sst", "dec%d" % par, "pD"], ["Sst"])
            cp("pool", Sbf[:], Sst[:], ["Sst"], ["Sbf"])
            yield
            for h in range(4):
                hs = slice(h * 128, (h + 1) * 128)
                mm(pD[:, hs], ke_tok[64:128, hs], vtk[64:128, hs], True, True, ["ke_tok", vtkk], ["pD"])
            for h in range(4):
                hs = slice(h * 128, (h + 1) * 128)
                mm(pO[64:128, hs], qd[:, h * 128 + 64:h * 128 + 128], Sbf[:, hs], False, True, [qdk, "Sbf"], ["pO"], skip_group_check=True)
            for h in range(4):
                hs = slice(h * 128, (h + 1) * 128)
                stt(Sst[:, hs], Sst[:, hs], dec[:, par * 8 + 2 * h + 1:par * 8 + 2 * h + 2], pD[:, hs], ALU.mult, ALU.add, ["Sst", "dec%d" % par, "pD"], ["Sst"])
            cp("pool", Sbf[:], Sst[:], ["Sst"], ["Sbf"])
            yield
            for h in range(4):
                hs = slice(h * 128, (h + 1) * 128)
                act(tmpB[:, hs], pO[:, hs], AF.Square, ["pO"], ["m2h", "sm8"], accum_out=sm[:, 8 + h:9 + h])
            ts("pool", sm[:, 8:12], sm[:, 8:12], 1.0 / 128, EPS, ALU.mult, ALU.add, ["sm8"], ["sm8"])
            tt("pool", sm[:, 8:12], sm[:, 8:12], nhalf[:, 0:4], ALU.pow, ["sm8", "nhalf"], ["sm8"])
            for h in range(4):
                hs = slice(h * 128, (h + 1) * 128)
                stt(tmpB[:, hs], pO[:, hs], sm[:, 8 + h:9 + h], ng_bc[:, hs], ALU.mult, ALU.mult, ["pO", "sm8", "ng_bc"], ["m2h"])
            tt("pool", yb[:], tmpB[:], sog[par][:], ALU.mult, ["m2h", "sog%d" % par], ["yb"])
            yield
            yield
            assert gdone.get(t)
            transpose8(yb, "yb", T4, "T4", n=4)
            yield
            for half in range(2):
                hs = slice(half * 512, (half + 1) * 512)
                bk_, bkk = nextM()
                for c in range(4):
                    mm(bk_[:], T4[:, c, :], wub[:, c, hs], c == 0, c == 3, ["T4", "wub"], [bkk])
                tt("dve", m2h[:], bk_[:], sgb[par][:, hs], ALU.mult, [bkk, "sgb%d" % par], ["m2h"])
                tt("pool", B8[:, hs], m2h[:], bigf2[:, hs], ALU.add, ["m2h", "bigf2"], ["B8"])
            yield
            yield
            transpose8(B8, "B8", T8, "T8")
            yield
            for half in range(2):
                hs = slice(half * 512, (half + 1) * 512)
                bk_, bkk = nextM()
                for kc in range(8):
                    mm(bk_[:], T8[:, kc, :], wo[:, kc, hs], kc == 0, kc == 7, ["T8", "wo"], [bkk])
                tt("dve", xb_[:, hs], bk_[:], xb_[:, hs], ALU.add, [bkk, xk], [xk])
            dma("sp", x1_d[t * 128:(t + 1) * 128, :], xb_[:], "x1st%d" % (t % 2), [xk], ["x1_d"])
            if dbg:
                dma("sp", dbg_d["x1"][t * 128:(t + 1) * 128, :], xb_[:], "dbgx1", [xk], [])
            yield
            rmsnorm_to_bf16(xb_, xk, gbc1, "gbc1", B8, "B8", 1)
            dma("sp", h2_d[t * 128:(t + 1) * 128, :], B8[:], "h2st", ["B8"], ["h2_d"])
            yield
            yield
            transpose8(B8, "B8", T8, "T8")
            yield
            for kc in range(8):
                mm(pA[:, 0:36], T8[:, kc, :], wr[:, kc, :], kc == 0, kc == 7, ["T8", "wr0", "wr1"], ["pA"])
            cp("dve", lg[:], pA[:, 0:36], ["pA"], ["lg"])
            yield
            cp("pool", gla[:, t, :], lg[:, 0:4], ["lg"], ["gla"])
            cp("dve", rt[:, 40:44], lg[:, 0:4], ["lg"], ["rt"])
            A("dve", lambda e: e.max(out=rt[:, 48:56], in_=rt[:, 40:48]), reads=["rt"], writes=["rt"])
            A("dve", lambda e: e.max_index(out=ridx[:, 0:8], in_max=rt[:, 48:56], in_values=rt[:, 40:48]), reads=["rt"], writes=["ridx"])
            cp("dve", gmxa[:, t:t + 1], rt[:, 48:49], ["rt"], ["gmxa"])
            cp("dve", rt[:, 56:57], ridx[:, 0:1], ["ridx"], ["rt"])
            ts("dve", rt[:, 0:4], io8[:, 0:4], rt[:, 56:57], None, ALU.is_equal, None, ["io8", "rt"], ["rt"])
            ts("dve", rt[:, 8:16], lg[:, 4:12], rt[:, 0:1], None, ALU.mult, None, ["lg", "rt"], ["rt"])
            for g in range(1, 4):
                stt(rt[:, 8:16], lg[:, 4 + g * 8:12 + g * 8], rt[:, g:g + 1], rt[:, 8:16], ALU.mult, ALU.add, ["lg", "rt"], ["rt"])
            A("dve", lambda e: e.max(out=rt[:, 16:24], in_=rt[:, 8:16]), reads=["rt"], writes=["rt"])
            A("dve", lambda e: e.max_index(out=ridx[:, 8:16], in_max=rt[:, 16:24], in_values=rt[:, 8:16]), reads=["rt"], writes=["ridx"])
            tt("dve", d12a[:, t:t + 1], rt[:, 16:17], rt[:, 17:18], ALU.subtract, ["rt"], ["d12a"])
            cp("dve", rt[:, 58:60], ridx[:, 8:10], ["ridx"], ["rt"])
            ts("dve", rt[:, 24:32], io8[:], rt[:, 58:59], None, ALU.is_equal, None, ["io8", "rt"], ["rt"])
            ts("dve", rt[:, 32:40], io8[:], rt[:, 59:60], None, ALU.is_equal, None, ["io8", "rt"], ["rt"])
            for g in range(4):
                ts("dve", M1a[:, t, g * 8:(g + 1) * 8], rt[:, 24:32], rt[:, g:g + 1], None, ALU.mult, None, ["rt"], ["M1a"])
                ts("pool", M2a[:, t, g * 8:(g + 1) * 8], rt[:, 32:40], rt[:, g:g + 1], None, ALU.mult, None, ["rt"], ["M2a"])
            tt("pool", Msum[par][:], M1a[:, t, :], M2a[:, t, :], ALU.add, ["M1a", "M2a"], ["Msum%d" % par])
            yield

        def rank_step(t):
            par = t % 2
            mm(pA[:, 64:96], stri_b[:], Msum[par][:], True, True, ["stri_b", "Msum%d" % par], ["pA"])
            mm(pA[:, 96:128], ones_b[:], Msum[par][:], True, True, ["ones_b", "Msum%d" % par], ["pA"])
            tt("dve", rka[:, t, :], pA[:, 64:96], runtot[:], ALU.add, ["pA", "runtot"], ["rka"])
            tt("dve", runtot[:], pA[:, 96:128], runtot[:], ALU.add, ["pA", "runtot"], ["runtot"])

        def interleave(gens):
            gens = [g for g in gens if g is not None]
            while gens:
                for g in list(gens):
                    try:
                        next(g)
                    except StopIteration:
                        gens.remove(g)

        if "A" in phases:
            interleave([front(0)])
            nz = NBLK
            zdone = 0
            for t in range(nt):
                for _ in range(2):
                    if zdone < nz and t >= 1:
                        zero_ops.append(dma("sp", xbuf_d[zdone * BR:(zdone + 1) * BR, :], zsrc_d[0:BR, :], "xzero", ["zsrc_d"], []))
                        zdone += 1
                fr_ = front(t + 1) if t + 1 < nt else None
                if KV == 1:
                    interleave([back(t), fr_, back_g(t)])
                elif KV == 2:
                    interleave([fr_, back(t), back_g(t)])
                else:
                    interleave([fr_, back_g(t), back(t)])
                if MODE == 1 and t + 1 < nt:
                    interleave([front(t + 1)])

        if "A" in phases:
            rank_step(nt - 1)
            while zdone < nz:
                zero_ops.append(dma("sp", xbuf_d[zdone * BR:(zdone + 1) * BR, :], zsrc_d[0:BR, :], "xzero", ["zsrc_d"], []))
                zdone += 1
        E1 = [0, 12288, 24576]
        OFF_PG = 0
        OFF_PLE = 8192
        if "B" in phases:
            lastA = [last_inproj[0]]
            BF3 = ["bigf3", "qdT1", "kiT1", "gv"]
            dma("pool", gbc0[:], g_ple_d.partition_broadcast(128), "gbc0", [], ["gbc0"])
            dma("sp", F1[0][:], g_final_d[:, 0:512].partition_broadcast(128), "gfin0", [], ["F1_0"])
            dma("sp", F2[:], g_final_d[:, 512:1024].partition_broadcast(128), "gfin1", [], ["F2"])
            tot = pb[:, 0, :]
            pad = pb[:, 1, :]
            pend = pb[:, 2, :]
            pstart = pb[:, 3, :]
            tmp32 = pb[:, 4, :]
            ones32 = pb[:, 5, :]
            memset("dve", ones32, 1.0, ["pb5"])
            cmp3 = bigf3[:].rearrange("p (e j) -> p e j", e=32)
            tt("dve", cmp3, runtot[:, :, None].to_broadcast([128, 32, 32]), Jt[:, None, 0:32].to_broadcast([128, 32, 32]),
               ALU.is_gt, ["runtot", "Jt"], BF3)
            A("dve", lambda e: e.tensor_reduce(out=pad, in_=cmp3, axis=AX.X, op=ALU.add), reads=["bigf3"], writes=["pb1"])
            ts("dve", pad, pad, float(BR), None, ALU.mult, None, ["pb1"], ["pb1"])
            A("dve", lambda e: e.tensor_tensor_scan(out=pend, data0=ones32, data1=pad, initial=0.0, op0=ALU.mult, op1=ALU.add),
              reads=["pb5", "pb1"], writes=["pb2"])
            tt("dve", pstart, pend, pad, ALU.subtract, ["pb2", "pb1"], ["pb3"])
            for bp in range(2):
                tt("dve", cmp3, pend[:, None, :].to_broadcast([128, 32, 32]), Jt[:, bp * 32:(bp + 1) * 32, None].to_broadcast([128, 32, 32]),
                   ALU.is_le, ["pb2", "Jt"], BF3)
                A("dve", lambda e, bp=bp: e.tensor_reduce(out=blkrow[:, bp * 32:(bp + 1) * 32], in_=cmp3, axis=AX.X, op=ALU.add),
                  reads=["bigf3"], writes=["blkrow"])
            ts("dve", blkrow[:], blkrow[:], 31.0, 128.0, ALU.min, ALU.mult, ["blkrow"], ["blkrow"])
            ts("dve", blkrow[:], blkrow[:], pid[:, 0:1], None, ALU.add, None, ["blkrow", "pid"], ["blkrow"])
            unu = bigf2[:, 0:96]
            ts("dve", unu, Jt[:, 0:96], pb[:, 2, 31:32], None, ALU.is_ge, None, ["Jt", "pb2"], ["bigf2"])
            ts("dve", unu, unu, pmask[:, 0:1], 4096.0, ALU.mult, ALU.mult, ["bigf2", "pmask"], ["bigf2"])
            tt("dve", blkrow[:, SKIP0:64], blkrow[:, SKIP0:64], unu[:, SKIP0:64], ALU.add, ["blkrow", "bigf2"], ["blkrow"])
            cp("dve", idxall[:], blkrow[:], ["blkrow"], ["idxall"])
            ts("dve", tmp32, pend, bstart[:, 0:1], None, ALU.is_le, None, ["pb2", "bstart"], ["pb4"])
            A("dve", lambda e: e.tensor_reduce(out=metaf[:, 0:1], in_=tmp32, axis=AX.X, op=ALU.add), reads=["pb4"], writes=["metaf"])
            ts("dve", metaf[:, 0:1], metaf[:, 0:1], 31.0, None, ALU.min, None, ["metaf"], ["metaf"])
            ts("dve", metaf[:, 1:2], pb[:, 2, 31:32], bstart[:, 0:1], None, ALU.is_gt, None, ["pb2", "bstart"], ["metaf"])
            cp("dve", metai[:], metaf[:], ["metaf"], ["metai"])
            dma("sp", meta_d, metai[:], "meta", ["metai"], ["meta_d"])
            if dbg:
                dma("sp", dbg_d["meta"], metai[:], "dbgm", ["metai"], [])
            pos3 = bigf3[:].rearrange("p (t e) -> p t e", t=NT)
            prd3 = bigf2[:].rearrange("p (t e) -> p t e", t=NT)
            tt("dve", pos3, rka[:], pstart[:, None, :].to_broadcast([128, NT, 32]), ALU.add, ["rka", "pb3"], BF3)
            tt("dve", prd3, M1a[:], pos3, ALU.mult, ["M1a", "bigf3"], ["bigf2"])
            A("dve", lambda e: e.tensor_reduce(out=d1f[:], in_=prd3, axis=AX.X, op=ALU.add), reads=["bigf2"], writes=["d1f"])
            tt("dve", prd3, M2a[:], pos3, ALU.mult, ["M2a", "bigf3"], ["bigf2"])
            A("dve", lambda e: e.tensor_reduce(out=d2f[:], in_=prd3, axis=AX.X, op=ALU.add), reads=["bigf2"], writes=["d2f"])
            cp("dve", dsti[:, 0:nt], d1f[:, 0:nt], ["d1f"], ["dsti"])
            cp("dve", dsti[:, NT:NT + nt], d2f[:, 0:nt], ["d2f"], ["dsti"])
            if dbg:
                dma("sp", dbg_d["dest"], dsti[:], "dbgd", ["dsti"], [])
            tt("dve", gla[:], gla[:], gmxa[:, :, None].to_broadcast([128, NT, 4]), ALU.subtract, ["gla", "gmxa"], ["gla"])
            act(gla[:], gla[:], AF.Exp, ["gla"], ["gla"])
            A("dve", lambda e: e.tensor_reduce(out=gmxa[:], in_=gla[:], axis=AX.X, op=ALU.add), reads=["gla"], writes=["gmxa"])
            A("dve", lambda e: e.reciprocal(out=gmxa[:], in_=gmxa[:]), reads=["gmxa"], writes=["gmxa"])
            act(d12a[:], d12a[:], AF.Sigmoid, ["d12a"], ["d12a"])
            tt("dve", g1a[:], d12a[:], gmxa[:], ALU.mult, ["d12a", "gmxa"], ["g1a"])
            tt("dve", g2a[:], gmxa[:], g1a[:], ALU.subtract, ["gmxa", "g1a"], ["g2a"])
            if dbg:
                dma("sp", dbg_d["gate"][:, 0:NT], g1a[:], "dbgg", ["g1a"], [])
                dma("sp", dbg_d["gate"][:, NT:2 * NT], g2a[:], "dbgg", ["g2a"], [])
            def emit_wload(b):
                j = b % 3
                base = E1[j]

                def wload(e, b=b, base=base):
                    off = bass.IndirectOffsetOnAxis(ap=idxall[:, b:b + 1], axis=0)
                    kw = dict(bounds_check=4095, oob_is_err=False) if b >= SKIP0 else {}
                    i0 = e.indirect_dma_start(out=wbig[:, base:base + 4096], out_offset=None,
                                              in_=w1_d[0].rearrange("e (p j) n -> (e p) (j n)", j=8), in_offset=off, **kw)
                    i1 = e.indirect_dma_start(out=wbig[:, base + 4096:base + 8192], out_offset=None,
                                              in_=w3_d[0].rearrange("e (p j) n -> (e p) (j n)", j=8), in_offset=off, **kw)
                    i2 = e.indirect_dma_start(out=wbig[:, base + 8192:base + 12288], out_offset=None,
                                              in_=w2_d[0].rearrange("e (p j) n -> (e p) (j n)", j=4), in_offset=off, **kw)
                    return [i0, i1, i2]
                op = A("pool", wload, reads=["idxall"], writes=["we_%d" % j], dma="we_%d" % j, n=3)
                if b < 3 and last_inproj[0] is not None:
                    op.deps.add(last_inproj[0]); last_inproj[0].has_dep = True
            for b0 in range(min(3, nblk)):
                emit_wload(b0)
            for t in range(nt):
                hb = B8 if t % 2 == 0 else B8b
                hk = "B8" if t % 2 == 0 else "B8b"
                dma("sp", hb[:], h2_d[t * 128:(t + 1) * 128, :], "h2ld%d" % (t % 2), ["h2_d"], [hk])
                for k in range(2):
                    col = k * NT + t
                    A("pool", lambda e, col=col, hb=hb: e.indirect_dma_start(
                        out=xbuf_d, out_offset=bass.IndirectOffsetOnAxis(ap=dsti[:, col:col + 1], axis=0), in_=hb[:], in_offset=None),
                      reads=[hk, "dsti"], writes=["xbuf_w%d_%d" % (k, t % 2)], dma="scat%d_%d" % (k, t % 2))
                    if t == 0:
                        S.ops[-1].deps.update(zero_ops)

        if "C" in phases:
            scat_ops = [op for op in S.ops if op.dma is not None and op.dma.startswith("scat")]
            NSB = BR // 128
            ABK = [((pM[0], "pM0"), (pM[1], "pM1")), ((pQ, "pQ"), (pF, "pF"))]

            XB3 = [(B8, "B8"), (B8b, "B8b"), (sga[0], "sga0")]

            def c0(q):
                b, sbi = divmod(q, NSB)
                r0 = b * BR + sbi * 128
                xb_sb, xbk = XB3[q % 3]
                op = dma("sp", xb_sb[:], xbuf_d[r0:r0 + 128, :], "xbld%d" % (q % 3), [], [xbk])
                if q < 3:
                    for so in scat_ops:
                        op.deps.add(so)

            def c1(q):
                b, sbi = divmod(q, NSB)
                j = b % 3
                base = E1[j]
                qp = q % 2
                r0 = b * BR + sbi * 128
                xb_sb, xbk = XB3[q % 3]
                T8q, T8qk = (T8, "T8") if qp == 0 else (T8f, "T8f")
                transpose8(xb_sb, xbk, T8q, T8qk, stride=8)
                (bA, bAk), (bB, bBk) = ABK[qp]
                for kc in range(8):
                    mm(bA[:], T8q[:, kc, :], wbig[:, base + kc * 512: base + (kc + 1) * 512], kc == 0, kc == 7, [T8qk, "we_%d" % j], [bAk])
                for kc in range(8):
                    mm(bB[:], T8q[:, kc, :], wbig[:, base + 4096 + kc * 512: base + 4096 + (kc + 1) * 512], kc == 0, kc == 7, [T8qk, "we_%d" % j], [bBk])

            def c2(q):
                b, sbi = divmod(q, NSB)
                j = b % 3
                base = E1[j]
                qp = q % 2
                r0 = b * BR + sbi * 128
                (bA, bAk), (bB, bBk) = ABK[qp]
                act(tmpA[:], bA[:], AF.Sigmoid, [bAk], ["tmpA"])
                tt("dve", tmpA[:], tmpA[:], bA[:], ALU.mult, ["tmpA", bAk], ["tmpA"])
                yq, yqk = (ya, "ya") if qp == 0 else (yb, "yb")
                tt("dve", yq[:], tmpA[:], bB[:], ALU.mult, ["tmpA", bBk], [yqk])
                transpose8(yq, yqk, T4, "T4", n=4, stride=4)
                ysb = xt[qp][:, 0:512].bitcast(BF16)
                yk = "xt%d" % qp
                for half, (bY, bYk) in enumerate(((pO, "pO"), (pD, "pD"))):
                    for c in range(4):
                        mm(bY[:], T4[:, c, :], wbig[:, base + 8192 + c * 1024 + half * 512: base + 8192 + c * 1024 + (half + 1) * 512],
                           c == 0, c == 3, ["T4", "we_%d" % j], [bYk])
                    cp("act" if half == 0 else "dve", ysb[:, half * 512:(half + 1) * 512], bY[:], [bYk], [yk])
                dma("sp", ybuf_d[r0:r0 + 128, :], ysb[:], "yst%d" % qp, [yk], ["ybuf_d"])

            nq = nblk * NSB
            c0(0)
            c0(1)
            c1(0)
            for q in range(nq):
                if q + 2 < nq:
                    c0(q + 2)
                if q + 1 < nq:
                    c1(q + 1)
                c2(q)
                if (q + 1) % NSB == 0:
                    nb_ = (q + 1) // NSB + 2
                    if nb_ < nblk:
                        emit_wload(nb_)

        if "D" in phases:
            dma("pool", wbig[:, OFF_PG:OFF_PG + 8192].rearrange("p (c n) -> p c n", c=8), w_pg_d[0].rearrange("(c p) n -> p c n", p=128),
                "wpg", [], ["wpg", "we_0"])
            dma("pool", wbig[:, OFF_PLE:OFF_PLE + 2048].rearrange("p (c n) -> p c n", c=2), w_ple_d[0].rearrange("(c p) n -> p c n", p=128),
                "wple", [], ["wple", "we_0"])
            yst_ops = [op for op in S.ops if op.dma is not None and op.dma.startswith("yst")]
            last_pe = next(op for op in reversed(S.ops) if op.eng == "pe")
            last_pe.has_dep = True
            BF3 = ["bigf3", "qdT1", "kiT1", "gv"]
            T8x = [T8, T8f]
            T8k = ["T8", "T8f"]
            WF = wbig[:, 12288:40960].bitcast(F32)
            xr3 = [xt[0][:], xt[1][:], WF[:, 0:1024], WF[:, 4096:5120]]
            xr3k = ["xt0", "xt1", "wf_x2", "wf_x3"]
            ygA = [bigf2[:, 0:512].bitcast(BF16), WF[:, 1024:1536].bitcast(BF16)]
            ygAk = [["bigf2"], ["wf_ya"]]
            ygB = [bigf3[:, 0:512].bitcast(BF16), WF[:, 1536:2048].bitcast(BF16)]
            ygBk = [BF3, ["wf_yb"]]
            ptl = [F3[:, 0:256], F3[:, 256:512]]
            ptlk = ["F3a", "F3b"]
            B8x = [B8, B8b]
            B8k = ["B8", "B8b"]
            ost = [(F4[:], "F4", F5[:], "F5"), (WF[:, 2048:2560], "wf_o0", WF[:, 2560:3072], "wf_o1")]
            junk = sga[0]
            B8x = [B8, B8b]
            first_wf = {}

            def wfdep(op, key):
                if key.startswith("wf_") and key not in first_wf:
                    first_wf[key] = True
                    op.deps.add(last_pe)
                return op

            def s1(t):
                i3 = t % 4
                par = t % 2
                xr, xk = xr3[i3], xr3k[i3]
                wfdep(dma("sp", xr, x1_d[t * 128:(t + 1) * 128, :], "x1ld%d" % i3, ["x1_d"], [xk]), xk)
                for k, (yb_, ybk) in enumerate(((ygA[par], ygAk[par]), (ygB[par], ygBk[par]))):
                    col = k * NT + t
                    og_ = A("pool", lambda e, col=col, yb_=yb_: e.indirect_dma_start(
                        out=yb_, out_offset=None, in_=ybuf_d, in_offset=bass.IndirectOffsetOnAxis(ap=dsti[:, col:col + 1], axis=0)),
                        reads=["dsti"], writes=ybk, dma="gath%d_%d" % (k, par))
                    wfdep(og_, ybk[0])
                    if t == 0:
                        for yo in yst_ops:
                            og_.deps.add(yo)
                dma("sp", ptl[par], p_d[t * 128:(t + 1) * 128, :], "pld%d" % par, [], [ptlk[par], "F3"])
                yield

            def s2(t):
                i3 = t % 4
                par = t % 2
                xr, xk = xr3[i3], xr3k[i3]
                stt(xr, ygA[par], g1a[:, t:t + 1], xr, ALU.mult, ALU.add, ygAk[par] + ["g1a", xk], [xk])
                stt(xr, ygB[par], g2a[:, t:t + 1], xr, ALU.mult, ALU.add, ygBk[par] + ["g2a", xk], [xk])
                if dbg:
                    dma("sp", dbg_d["x2"][t * 128:(t + 1) * 128, :], xr, "dbgx2", [xk], [])
                cp("act", ptbx[par], ptl[par], [ptlk[par]], ["ptb%d" % par])
                yield
                rmsnorm_to_bf16(xr, xk, gbc0, "gbc0", B8x[par], B8k[par], 2)
                yield

            def s2b(t):
                par = t % 2
                transpose8(B8x[par], B8k[par], T8x[par], T8k[par])
                yield
                for c in range(2):
                    tr(pT[:, c * 128:(c + 1) * 128], ptbx[par][:, c * 128:(c + 1) * 128], ident_b[:], ["ptb%d" % par, "ident_b"], ["pT"])
                cp("act", T4[:, 2 * par:2 * par + 2, :].rearrange("p c t -> p (c t)"), pT[:, 0:256], ["pT"], ["T4_%d" % par, "T4"])
                yield

            def s3(t):
                i3 = t % 4
                par = t % 2
                xr, xk = xr3[i3], xr3k[i3]
                for half in range(2):
                    hs = slice(half * 512, (half + 1) * 512)
                    bG, bGk = nextM()
                    for kc in range(8):
                        mm(bG[:], T8x[par][:, kc, :], wbig[:, OFF_PG + kc * 1024 + half * 512: OFF_PG + kc * 1024 + (half + 1) * 512],
                           kc == 0, kc == 7, [T8k[par], "wpg"], [bGk])
                    act(tmpA[:], bG[:], AF.Sigmoid, [bGk], ["tmpA"])
                    bP, bPk = (pO, "pO") if half == 0 else (pD, "pD")
                    for c in range(2):
                        mm(bP[:], T4[:, 2 * par + c, :], wbig[:, OFF_PLE + c * 1024 + half * 512: OFF_PLE + c * 1024 + (half + 1) * 512],
                           c == 0, c == 1, ["T4_%d" % par, "wple"], [bPk])
                    tt("dve", tmpA[:], tmpA[:], bP[:], ALU.mult, ["tmpA", bPk], ["tmpA"])
                    tt("dve", xr[:, hs], xr[:, hs], tmpA[:], ALU.add, [xk, "tmpA"], [xk])
                    yield
                act(junk[:], xr, AF.Square, [xk], ["sga0", "sm3"], accum_out=sm[:, 3:4])
                ts("pool", sm[:, 3:4], sm[:, 3:4], 1.0 / D, EPS, ALU.mult, ALU.add, ["sm3"], ["sm3"])
                tt("pool", sm[:, 3:4], sm[:, 3:4], nhalf[:, 0:1], ALU.pow, ["sm3", "nhalf"], ["sm3"])
                yield
                o0, o0k, o1, o1k = ost[par]
                wfdep(stt(o0, xr[:, 0:512], sm[:, 3:4], F1[0][:], ALU.mult, ALU.mult, [xk, "sm3", "F1_0"], [o0k]), o0k)
                wfdep(stt(o1, xr[:, 512:1024], sm[:, 3:4], F2[:], ALU.mult, ALU.mult, [xk, "sm3", "F2"], [o1k]), o1k)
                dma("sp", out_d[t * 128:(t + 1) * 128, 0:512], o0, "outst0_%d" % par, [o0k], ["out_d0"])
                dma("sp", out_d[t * 128:(t + 1) * 128, 512:1024], o1, "outst1_%d" % par, [o1k], ["out_d1"])
                yield

            ptbx = [ptb[:, 0:256], WF[:, 3072:3200].bitcast(BF16)]
            for step in range(nt + 3):
                interleave([s3(step - 3) if 0 <= step - 3 < nt else None,
                            s2b(step - 2) if 0 <= step - 2 < nt else None,
                            s2(step - 1) if 0 <= step - 1 < nt else None,
                            s1(step) if step < nt else None])
        S.emit(nc)
    return nc


_CACHE = {}


def kernel(**inputs):
    names = ["g_mix", "w_in", "gm_ln_g", "gm_ln_b", "gm_w_sp", "gm_b_sp", "w_up_a", "hg_lb_param", "hg_norm_g", "w_up_b",
             "w_out", "g_ffn", "w_grp", "w_exp", "w1", "w3", "w2", "g_ple", "w_pg", "w_ple"]
    x = np.ascontiguousarray(np.asarray(inputs["x"], dtype=np.float32)).reshape(NCORES, TOK, D)
    p = np.ascontiguousarray(np.asarray(inputs["p"], dtype=np.float32)).reshape(NCORES, TOK, 256)
    shared = {n: np.ascontiguousarray(np.asarray(inputs[n], dtype=np.float32)) for n in names}
    shared["g_final"] = np.ascontiguousarray(np.asarray(inputs["g_final"], dtype=np.float32)).reshape(1, D)
    if "nc" not in _CACHE:
        _CACHE["nc"] = build()
    nc = _CACHE["nc"]
    in_maps = []
    for c in range(NCORES):
        m = dict(shared)
        m["x"] = x[c]
        m["p"] = p[c]
        in_maps.append(m)
    res = run_bass_kernel_spmd(nc, in_maps, core_ids=list(range(NCORES)))
    out = np.stack([np.asarray(r["out"], dtype=np.float32) for r in res.results], axis=0)
    return out.reshape(16, 2048, D)
```

```python
import contextlib
import numpy as np
import concourse.bass as bass
import concourse.mybir as mybir
from concourse.bass_utils import run_bass_kernel_spmd

F32 = mybir.dt.float32
BF16 = mybir.dt.bfloat16
I32 = mybir.dt.int32
ALU = mybir.AluOpType
AF = mybir.ActivationFunctionType
AX = mybir.AxisListType

import os
MODE = 2
KV = int(os.environ.get('KV', '1'))
NCORES = 8
TOK = 4096
NT = TOK // 128
TPS = 16
D = 1024
NBLK = 64
BR = 256
SKIP0 = 51
EPS = 1e-6
EPOCH = 4000
ENGS = ("pe", "act", "dve", "pool", "sp")


class Op:
    __slots__ = ("eng", "fn", "deps", "has_dep", "dma", "sem", "val", "idx", "force", "n")

    def __init__(self, eng, fn, dma, idx):
        self.eng = eng
        self.fn = fn
        self.dma = dma
        self.deps = set()
        self.has_dep = False
        self.sem = None
        self.val = 0
        self.idx = idx
        self.force = False
        self.n = 1


class Sched:
    def __init__(self):
        self.ops = []
        self.last_w = {}
        self.readers = {}

    def add(self, eng, fn, reads=(), writes=(), dma=None, after=(), n=1):
        op = Op(eng, fn, dma, len(self.ops))
        op.n = n
        deps = set(a for a in after if a is not None)
        for k in reads:
            w = self.last_w.get(k)
            if w is not None:
                deps.add(w)
        for k in writes:
            w = self.last_w.get(k)
            if w is not None:
                deps.add(w)
            for r in self.readers.get(k, ()):
                deps.add(r)
        for k in reads:
            self.readers.setdefault(k, []).append(op)
        for k in writes:
            self.last_w[k] = op
            self.readers[k] = []
        deps.discard(op)
        if eng == "pe":
            deps = set(d for d in deps if d.eng != "pe" or d.dma is not None)
        op.deps = deps
        for d in deps:
            d.has_dep = True
        self.ops.append(op)
        return op

    def emit(self, nc, final_wait_eng="sp"):
        cnt = {e: 0 for e in ENGS}
        dma_cnt = {}
        sem_names = set()
        for op in self.ops:
            if op.dma is not None:
                dma_cnt[op.dma] = dma_cnt.get(op.dma, 0) + op.n
                op.sem = "dma_" + op.dma
                op.val = 16 * dma_cnt[op.dma]
                sem_names.add(op.sem)
            elif op.has_dep:
                c = cnt[op.eng]
                cnt[op.eng] = c + 1
                op.sem = "c_%s_%d" % (op.eng, c // EPOCH)
                op.val = (c % EPOCH) + 1
                sem_names.add(op.sem)
        sem_names = sorted(sem_names)
        self.n_sems = len(sem_names)
        with contextlib.ExitStack() as st:
            sems = {n: st.enter_context(nc.semaphore(n)) for n in sem_names}
            block = st.enter_context(nc.Block())
            final = {}
            for op in self.ops:
                if op.dma is not None:
                    final[op.sem] = max(final.get(op.sem, 0), op.val)

            def run(engname, eng):
                waited = {}
                for op in self.ops:
                    if op.eng != engname:
                        continue
                    need = {}
                    for d in op.deps:
                        if waited.get(d.sem, 0) < d.val:
                            need[d.sem] = max(need.get(d.sem, 0), d.val)
                    for s, v in need.items():
                        eng.wait_ge(sems[s], v)
                        waited[s] = v
                    inst = op.fn(eng)
                    if op.sem is not None:
                        insts = inst if isinstance(inst, (list, tuple)) else [inst]
                        for ii in insts:
                            ii.then_inc(sems[op.sem], 16 if op.dma is not None else 1)
                if engname == final_wait_eng:
                    for s, v in final.items():
                        if waited.get(s, 0) < v:
                            eng.wait_ge(sems[s], v)

            @block.tensor
            def _(e):
                run("pe", e)

            @block.scalar
            def _(e):
                run("act", e)

            @block.vector
            def _(e):
                run("dve", e)

            @block.gpsimd
            def _(e):
                run("pool", e)

            @block.sync
            def _(e):
                run("sp", e)


def build(dbg=False, nt=NT, nblk=NBLK, phases="ABCD"):
    nc = bass.Bass("TRN2", target_bir_lowering=False)
    S = Sched()
    st = contextlib.ExitStack()

    def din(name, shape, dt=F32):
        return nc.dram_tensor(name, list(shape), dt, kind="ExternalInput").ap()

    x_d = din("x", [TOK, D])
    p_d = din("p", [TOK, 256])
    g_mix_d = din("g_mix", [1, D])
    w_in_d = din("w_in", [1, D, 5120])
    ln_g_d = din("gm_ln_g", [1, 512])
    ln_b_d = din("gm_ln_b", [1, 512])
    w_sp_d = din("gm_w_sp", [1, 8, 128, 128])
    b_sp_d = din("gm_b_sp", [1, 8, 128])
    w_up_a_d = din("w_up_a", [1, 512, D])
    lbp_d = din("hg_lb_param", [2, 512])
    ng_d = din("hg_norm_g", [1, 512])
    w_up_b_d = din("w_up_b", [1, 512, D])
    w_out_d = din("w_out", [1, D, D])
    g_ffn_d = din("g_ffn", [1, D])
    w_grp_d = din("w_grp", [1, D, 4])
    w_exp_d = din("w_exp", [1, D, 32])
    w1_d = din("w1", [1, 32, D, 512])
    w3_d = din("w3", [1, 32, D, 512])
    w2_d = din("w2", [1, 32, 512, D])
    g_ple_d = din("g_ple", [1, D])
    w_pg_d = din("w_pg", [1, D, D])
    w_ple_d = din("w_ple", [1, 256, D])
    g_final_d = din("g_final", [1, D])
    out_d = nc.dram_tensor("out", [TOK, D], F32, kind="ExternalOutput").ap()
    x1_d = nc.dram_tensor("x1_scr", [TOK, D], F32, kind="Internal").ap()
    h2_d = nc.dram_tensor("h2_scr", [TOK, D], BF16, kind="Internal").ap()
    xbuf_d = nc.dram_tensor("xbuf_scr", [NBLK * BR, D], BF16, kind="Internal").ap()
    ybuf_d = nc.dram_tensor("ybuf_scr", [NBLK * BR, D], BF16, kind="Internal").ap()
    meta_d = nc.dram_tensor("meta_scr", [128, 2], I32, kind="Internal").ap()
    zsrc_d = nc.dram_tensor("zsrc_scr", [512, D], BF16, kind="Internal").ap()
    dbg_d = {}
    if dbg:
        dbg_d["x1"] = nc.dram_tensor("dbg_x1", [TOK, D], F32, kind="ExternalOutput").ap()
        dbg_d["meta"] = nc.dram_tensor("dbg_meta", [128, 2], I32, kind="ExternalOutput").ap()
        dbg_d["dest"] = nc.dram_tensor("dbg_dest", [128, 64], I32, kind="ExternalOutput").ap()
        dbg_d["gate"] = nc.dram_tensor("dbg_gate", [128, 64], F32, kind="ExternalOutput").ap()
        dbg_d["x2"] = nc.dram_tensor("dbg_x2", [TOK, D], F32, kind="ExternalOutput").ap()

    def sb(name, shape, dt):
        return st.enter_context(nc.sbuf_tensor(name, list(shape), dt))

    def ps(name, shape, dt):
        return st.enter_context(nc.psum_tensor(name, list(shape), dt))

    wbig = sb("wbig", [128, 40960], BF16)
    wua = sb("wua", [128, 4, D], BF16)
    wub = sb("wub", [128, 4, D], BF16)
    wo = sb("wo", [128, 8, D], BF16)
    ident_b = sb("ident_b", [128, 128], BF16)
    ident_f = sb("ident_f", [128, 128], F32)
    ones_f = sb("ones_f", [128, 128], F32)
    ones_b = sb("ones_b", [128, 128], BF16)
    stri_b = sb("stri_b", [128, 128], BF16)
    tri_f = sb("tri_f", [128, 128], F32)
    hmask4 = sb("hmask4", [128, 512], BF16)
    wspT = sb("wspT", [128, 8, 128], BF16)
    bsp = sb("bsp", [128, 8], F32)
    lng_bc = sb("lng_bc", [128, 512], BF16)
    lnb_bc = sb("lnb_bc", [128, 512], BF16)
    ng_bc = sb("ng_bc", [128, 512], BF16)
    gbc0 = sb("gbc0", [128, D], BF16)
    gbc1 = sb("gbc1", [128, D], BF16)
    wr = sb("wr", [128, 8, 36], BF16)
    rmask = sb("rmask", [128, 512], BF16)
    pmask = sb("pmask", [128, 1], F32)
    lbp = sb("lbp", [128, 2, 4], F32)
    lb = sb("lb", [128, 4], F32)
    oml = sb("oml", [128, 4], F32)
    nhalf = sb("nhalf", [128, 4], F32)
    bstart = sb("bstart", [128, 1], F32)
    Jt = sb("Jt", [128, 96], F32)
    pid = sb("pid", [128, 1], F32)
    blkrow = sb("blkrow", [128, 96], F32)
    idxall = sb("idxall", [128, 96], I32)

    xt = [sb("xt0", [128, D], F32), sb("xt1", [128, D], F32)]
    bigf2 = sb("bigf2", [128, D], F32)
    bigf3 = sb("bigf3", [128, D], F32)
    B8 = sb("B8", [128, D], BF16)
    B8b = sb("B8b", [128, D], BF16)
    T8 = sb("T8", [128, 8, 128], BF16)
    T8f = sb("T8f", [128, 8, 128], BF16)
    T4 = sb("T4", [128, 4, 128], BF16)
    tmpA = sb("tmpA", [128, 512], F32)
    gu = [sb("gu0", [128, 512], BF16), sb("gu1", [128, 512], BF16)]
    gv = bigf3[:, 512:1024]
    vn = [sb("vn0", [128, 512], BF16), sb("vn1", [128, 512], BF16)]
    ya = sb("ya", [128, 512], BF16)
    v_tok = [sb("v_tok0", [128, 512], BF16), sb("v_tok1", [128, 512], BF16)]
    sog = [sb("sog0", [128, 512], BF16), sb("sog1", [128, 512], BF16)]
    sga = [sb("sga0", [128, D], BF16), sb("sga1", [128, D], BF16)]
    sgb = [sb("sgb0", [128, D], BF16), sb("sgb1", [128, D], BF16)]
    F1 = [sb("F1_0", [128, 512], F32)]
    F2 = sb("F2", [128, 512], F32)
    F3 = sb("F3", [128, 512], F32)
    F4 = sb("F4", [128, 512], F32)
    F5 = sb("F5", [128, 512], F32)
    F6 = [sb("F6_0", [128, 512], BF16), sb("F6_1", [128, 512], BF16)]
    _al = bigf3[:, 0:512].bitcast(BF16)
    qdT = [sb("qdT0", [128, 512], BF16), _al[:, 0:512]]
    kiT = [sb("kiT0", [128, 512], BF16), _al[:, 512:1024]]
    keT = [sb("keT0", [128, 512], BF16), F6[1]]
    dec = sb("dec", [128, 16], F32)
    ke_tok = sb("ke_tok", [128, 512], BF16)
    attT = sb("attT", [128, 512], BF16)
    Sst = sb("Sst", [128, 512], F32)
    Sbf = sb("Sbf", [128, 512], BF16)
    yb = sb("yb", [128, 512], BF16)
    m2h = sb("m2h", [128, 512], F32)
    tmpB = m2h
    st6 = sb("st6", [128, 6], F32)
    sm = sb("sm", [128, 32], F32)
    lg = sb("lg", [128, 36], F32)
    rt = sb("rt", [128, 64], F32)
    ridx = sb("ridx", [128, 16], mybir.dt.uint32)
    io8 = sb("io8", [128, 8], F32)
    M1a = sb("M1a", [128, NT, 32], BF16)
    M2a = sb("M2a", [128, NT, 32], BF16)
    Msum = [sb("Msum0", [128, 32], BF16), sb("Msum1", [128, 32], BF16)]
    rka = sb("rka", [128, NT, 32], F32)
    gla = sb("gla", [128, NT, 4], F32)
    gmxa = sb("gmxa", [128, NT], F32)
    d12a = sb("d12a", [128, NT], F32)
    g1a = sb("g1a", [128, NT], F32)
    g2a = sb("g2a", [128, NT], F32)
    runtot = sb("runtot", [128, 32], F32)
    pb = sb("pb", [128, 6, 32], F32)
    pbi = sb("pbi", [128, 32], I32)
    d1f = sb("d1f", [128, NT], F32)
    d2f = sb("d2f", [128, NT], F32)
    dsti = sb("dsti", [128, 2 * NT], I32)
    metaf = sb("metaf", [128, 2], F32)
    metai = sb("metai", [128, 2], I32)
    ptile = F3[:, 0:256]
    ptb = sb("ptb", [128, 256], BF16)

    pT = ps("pT", [128, 1024], BF16)
    pQ = ps("pQ", [128, 512], F32)
    pF = ps("pF", [128, 512], F32)
    pM = [ps("pM0", [128, 512], F32), ps("pM1", [128, 512], F32)]
    pO = ps("pO", [128, 512], F32)
    pD = ps("pD", [128, 512], F32)
    pA = ps("pA", [128, 512], F32)
    mrot = [0]

    def nextM():
        i = mrot[0] % 2
        mrot[0] += 1
        return pM[i], "pM%d" % i

    A = S.add

    def mm(out, lhsT, rhs, start, stop, reads, writes, **kw):
        return A("pe", lambda e: e.matmul(out=out, lhsT=lhsT, rhs=rhs, start=start, stop=stop, **kw), reads=reads, writes=writes)

    def tr(out, in_, ident, reads, writes):
        return A("pe", lambda e: e.transpose(out=out, in_=in_, identity=ident), reads=reads, writes=writes)

    def act(out, in_, func, reads, writes, **kw):
        return A("act", lambda e: e.activation(out=out, in_=in_, func=func, **kw), reads=reads, writes=writes)

    def tt(eng, out, in0, in1, op, reads, writes):
        return A(eng, lambda e: e.tensor_tensor(out=out, in0=in0, in1=in1, op=op), reads=reads, writes=writes)

    def ts(eng, out, in0, s1, s2, op0, op1, reads, writes):
        if s2 is None:
            return A(eng, lambda e: e.tensor_scalar(out=out, in0=in0, scalar1=s1, scalar2=None, op0=op0), reads=reads, writes=writes)
        return A(eng, lambda e: e.tensor_scalar(out=out, in0=in0, scalar1=s1, scalar2=s2, op0=op0, op1=op1), reads=reads, writes=writes)

    def stt(out, in0, scalar, in1, op0, op1, reads, writes, accum=None):
        if accum is None:
            return A("dve", lambda e: e.scalar_tensor_tensor(out=out, in0=in0, scalar=scalar, in1=in1, op0=op0, op1=op1), reads=reads, writes=writes)
        return A("dve", lambda e: e.scalar_tensor_tensor(out=out, in0=in0, scalar=scalar, in1=in1, op0=op0, op1=op1, accum_out=accum), reads=reads, writes=writes)

    def cp(eng, out, in_, reads, writes):
        if eng == "act":
            return A("act", lambda e: e.copy(out=out, in_=in_), reads=reads, writes=writes)
        return A(eng, lambda e: e.tensor_copy(out=out, in_=in_), reads=reads, writes=writes)

    def dma(q, out, in_, key, reads, writes, **kw):
        return A(q, lambda e: e.dma_start(out=out, in_=in_, **kw), reads=reads, writes=writes, dma=key)

    def memset(eng, ap, val, writes):
        return A(eng, lambda e: e.memset(ap, val), writes=writes)

    with st:
        for kc in range(8):
            dma("pool", wbig[:, kc * 5120:(kc + 1) * 5120], w_in_d[0, kc * 128:(kc + 1) * 128, :], "win%d" % kc, [], ["win%d" % kc])
        memset("pool", ones_f[:], 1.0, ["ones_f"])
        memset("pool", ones_b[:], 1.0, ["ones_b"])
        memset("pool", nhalf[:], -0.5, ["nhalf"])
        A("pool", lambda e: e.affine_select(out=ident_f[:], in_=ones_f[:], pattern=[[1, 128]], compare_op=ALU.is_equal,
                                            fill=0.0, base=0, channel_multiplier=-1), reads=["ones_f"], writes=["ident_f"])
        A("pool", lambda e: e.affine_select(out=ident_b[:], in_=ones_f[:], pattern=[[1, 128]], compare_op=ALU.is_equal,
                                            fill=0.0, base=0, channel_multiplier=-1), reads=["ones_f"], writes=["ident_b"])
        A("pool", lambda e: e.affine_select(out=tri_f[:], in_=ones_f[:], pattern=[[1, 128]], compare_op=ALU.is_ge,
                                            fill=0.0, base=0, channel_multiplier=-1), reads=["ones_f"], writes=["tri_f"])
        A("pool", lambda e: e.affine_select(out=stri_b[:], in_=ones_f[:], pattern=[[1, 128]], compare_op=ALU.is_gt,
                                            fill=0.0, base=0, channel_multiplier=-1), reads=["ones_f"], writes=["stri_b"])
        for h in range(4):
            cp("pool", hmask4[:, h * 128:(h + 1) * 128], tri_f[:], ["tri_f"], ["hmask4"])
        for h in range(4):
            memset("pool", hmask4[0:64, h * 128 + 64:(h + 1) * 128], 0.0, ["hmask4"])
        memset("pool", rmask[:], 1.0, ["rmask"])
        for c in range(0, 512, 64):
            memset("pool", rmask[:, c:c + 1], 0.0, ["rmask"])
        A("pool", lambda e: e.iota(out=bstart[:], pattern=[[0, 1]], base=0, channel_multiplier=BR,
                                   allow_small_or_imprecise_dtypes=True), writes=["bstart"])
        memset("pool", runtot[:], 0.0, ["runtot"])
        memset("pool", pmask[:], 1.0, ["pmask"])
        memset("pool", blkrow[:], 0.0, ["blkrow"])
        memset("pool", rt[:], 0.0, ["rt"])
        memset("pool", rt[:, 44:48], -1.0e30, ["rt"])
        memset("pool", ridx[:], 0, ["ridx"])
        A("pool", lambda e: e.iota(out=io8[:], pattern=[[1, 8]], base=0, channel_multiplier=0,
                                   allow_small_or_imprecise_dtypes=True), writes=["io8"])
        memset("pool", sm[:], 0.0, ["sm0", "sm1", "sm2", "sm3", "sm4", "sm6", "sm8"])
        memset("pool", pb[:], 0.0, ["pb0", "pb1", "pb2", "pb3", "pb4", "pb5"])
        memset("pool", dec[:], 0.0, ["dec0", "dec1"])
        memset("pool", pmask[0:1, :], 0.0, ["pmask"])
        A("pool", lambda e: e.iota(out=pid[:], pattern=[[0, 1]], base=0, channel_multiplier=1,
                                   allow_small_or_imprecise_dtypes=True), writes=["pid"])
        A("pool", lambda e: e.iota(out=Jt[:], pattern=[[BR, 96]], base=0, channel_multiplier=0,
                                   allow_small_or_imprecise_dtypes=True), writes=["Jt"])

        memset("pool", bigf2[:], 0.0, ["bigf2"])
        zsrc = bigf2[:].bitcast(BF16).rearrange("p (r d) -> p r d", r=2)
        zero_ops = []
        for zi in range(2):
            dma("sp", zsrc_d[zi * 256:(zi + 1) * 256, :].rearrange("(r p) d -> p r d", p=128), zsrc, "zsrc", ["bigf2"], ["zsrc_d"])
        WIN = ["win%d" % k for k in range(8)]
        dma("pool", wua[:], w_up_a_d[0].rearrange("(c p) n -> p c n", p=128), "wua", [], ["wua"])
        dma("pool", wub[:], w_up_b_d[0].rearrange("(c p) n -> p c n", p=128), "wub", [], ["wub"])
        dma("pool", wo[:], w_out_d[0].rearrange("(c p) n -> p c n", p=128), "wo", [], ["wo"])
        dma("pool", wr[:, :, 0:4], w_grp_d[0].rearrange("(c p) n -> p c n", p=128), "wr0", [], ["wr0"])
        dma("pool", wr[:, :, 4:36], w_exp_d[0].rearrange("(c p) n -> p c n", p=128), "wr1", [], ["wr1"])
        dma("pool", gbc0[:], g_mix_d.partition_broadcast(128), "gbc0", [], ["gbc0"])
        dma("pool", lng_bc[:], ln_g_d.partition_broadcast(128), "lng", [], ["lng_bc"])
        dma("pool", lnb_bc[:], ln_b_d.partition_broadcast(128), "lnb", [], ["lnb_bc"])
        dma("pool", ng_bc[:], ng_d.partition_broadcast(128), "ngb", [], ["ng_bc"])
        dma("pool", gbc1[:], g_ffn_d.partition_broadcast(128), "gbc1", [], ["gbc1"])
        dma("sp", bsp[:], b_sp_d[0].rearrange("g t -> t g"), "bsp", [], ["bsp"], allow_slow_non_contiguous=True)
        dma("sp", lbp[:], lbp_d.rearrange("s (h k) -> k s h", k=128), "lbp", [], ["lbp"], allow_slow_non_contiguous=True)
        tt("dve", lb[:], lbp[:, 0, :], lbp[:, 1, :], ALU.subtract, ["lbp"], ["lb"])
        act(lb[:], lb[:], AF.Sigmoid, ["lb"], ["lb"])
        ts("dve", oml[:], lb[:], -1.0, 1.0, ALU.mult, ALU.add, ["lb"], ["oml"])
        wraw = bigf2
        dma("sp", wraw[:].rearrange("p (g s) -> p g s", g=8), w_sp_d[0].rearrange("g t s -> t g s"), "wraw", [], ["bigf2"])
        for half in range(2):
            for j in range(4):
                g = half * 4 + j
                tr(pQ[:, j * 128:(j + 1) * 128], wraw[:, g * 128:(g + 1) * 128], ident_f[:], ["bigf2", "ident_f"], ["pQ"])
            for j in range(4):
                g = half * 4 + j
                tt("dve", wspT[:, g, :], pQ[:, j * 128:(j + 1) * 128], tri_f[:], ALU.mult, ["pQ", "tri_f"], ["wspT"])

        def rmsnorm_to_bf16(src, src_key, gbc, gkey, dst, dst_key, smcol):
            act(dst[:], src[:], AF.Square, [src_key], [dst_key, "sm%d" % smcol], accum_out=sm[:, smcol:smcol + 1])
            ts("pool", sm[:, smcol:smcol + 1], sm[:, smcol:smcol + 1], 1.0 / D, EPS, ALU.mult, ALU.add, ["sm%d" % smcol], ["sm%d" % smcol])
            tt("pool", sm[:, smcol:smcol + 1], sm[:, smcol:smcol + 1], nhalf[:, 0:1], ALU.pow, ["sm%d" % smcol, "nhalf"], ["sm%d" % smcol])
            stt(dst[:], src[:], sm[:, smcol:smcol + 1], gbc[:], ALU.mult, ALU.mult, [src_key, "sm%d" % smcol, gkey], [dst_key])

        def transpose8(src, src_key, dst, dst_key, n=8, stride=None):
            for kc in range(n):
                if stride is None:
                    sl = src[:, kc * 128:(kc + 1) * 128]
                else:
                    sl = src[:, kc:128 * stride:stride]
                tr(pT[:, kc * 128:(kc + 1) * 128], sl, ident_b[:], [src_key, "ident_b"], ["pT"])
            cp("act", dst[:, 0:n, :].rearrange("p c t -> p (c t)") if False else dst[:].rearrange("p c t -> p (c t)"), pT[:, 0:n * 128], ["pT"], [dst_key])

        def gelu_from_psum(pbank, pkey, dst, dst_key):
            act(tmpA[:], pbank[:], AF.Square, [pkey], ["tmpA"], scale=0.21145921592579985)
            stt(tmpA[:], tmpA[:], 1.0, pbank[:], ALU.add, ALU.mult, ["tmpA", pkey], ["tmpA"])
            act(tmpA[:], tmpA[:], AF.Sigmoid, ["tmpA"], ["tmpA"], scale=1.5957691216057308)
            tt("dve", dst[:], tmpA[:], pbank[:], ALU.mult, ["tmpA", pkey], [dst_key])

        last_inproj = [None]

        def front(t):
            par = t % 2
            xb_ = xt[par]
            xk = "xt%d" % par
            dma("sp", xb_[:], x_d[t * 128:(t + 1) * 128, :], xk, [], [xk])
            yield
            rmsnorm_to_bf16(xb_, xk, gbc0, "gbc0", B8b, "B8b", 0)
            yield
            yield
            transpose8(B8b, "B8b", T8f, "T8f")
            hT = T8f
            yield
            for (pb_, pk, c0) in ((pF, "pF", 1536), (pQ, "pQ", 1024)):
                for h in range(4):
                    for kc in range(8):
                        last_inproj[0] = mm(pb_[:, h * 128:(h + 1) * 128], wbig[:, kc * 5120 + c0 + h * 128: kc * 5120 + c0 + (h + 1) * 128],
                                            hT[:, kc, :], kc == 0, kc == 7, ["T8f"] + WIN, [pk])
                if pk == "pQ":
                    act(F6[0][:], pQ[:], AF.Sigmoid, ["pQ"], ["F6_0"])
                    tt("dve", F6[0][:], F6[0][:], pQ[:], ALU.mult, ["F6_0", "pQ"], ["F6_0"])
                else:
                    act(F1[0][:], pF[:], AF.Sigmoid, ["pF"], ["F1_0"])
                yield

            def inproj_tok(c0):
                bank, bk = nextM()
                for kc in range(8):
                    last_inproj[0] = mm(bank[:], hT[:, kc, :], wbig[:, kc * 5120 + c0: kc * 5120 + c0 + 512], kc == 0, kc == 7, ["T8f"] + WIN, [bk])
                return bank, bk

            bv, bvk = inproj_tok(512)
            gelu_from_psum(bv, bvk, gv, "gv")
            yield
            F1p, F1k = F1[0], "F1_0"
            for h in range(4):
                act(F1p[:, h * 128:(h + 1) * 128], F1p[:, h * 128:(h + 1) * 128], AF.Identity, [F1k, "oml", "lb"], [F1k],
                    scale=oml[:, h:h + 1], bias=lb[:, h:h + 1])
            ts("pool", F2[:], F1p[:], -1.0, 1.0, ALU.mult, ALU.add, [F1k], ["F2"])
            tt("pool", F3[:], F1p[:], rmask[:], ALU.mult, [F1k, "rmask"], ["F3"])
            tt("pool", F4[:], F1p[:], F3[:], ALU.subtract, [F1k, "F3"], ["F4"])
            yield
            A("dve", lambda e: e.bn_stats(out=st6[:], in_=gv[:]), reads=["gv"], writes=["st6"])
            A("dve", lambda e: e.bn_aggr(out=sm[:, 4:6], in_=st6[:]), reads=["st6"], writes=["sm4"])
            ts("pool", sm[:, 6:7], sm[:, 5:6], EPS, None, ALU.add, None, ["sm4"], ["sm6"])
            tt("pool", sm[:, 6:7], sm[:, 6:7], nhalf[:, 0:1], ALU.pow, ["sm6", "nhalf"], ["sm6"])
            ts("dve", gv[:], gv[:], sm[:, 4:5], sm[:, 6:7], ALU.subtract, ALU.mult, ["gv", "sm4", "sm6"], ["gv"])
            tt("pool", gv[:], gv[:], lng_bc[:], ALU.mult, ["gv", "lng_bc"], ["gv"])
            tt("pool", vn[par][:], gv[:], lnb_bc[:], ALU.add, ["gv", "lnb_bc"], ["vn%d" % par])
            yield
            bu, buk = inproj_tok(0)
            gelu_from_psum(bu, buk, gu[par], "gu%d" % par)
            yield
            A("dve", lambda e: e.tensor_tensor_scan(out=F5[:], data0=F3[:], data1=F4[:], initial=0.0, op0=ALU.mult, op1=ALU.add),
              reads=["F3", "F4"], writes=["F5"])
            A("dve", lambda e: e.reciprocal(out=F3[:], in_=F5[:]), reads=["F5"], writes=["F3"])
            tt("dve", qdT[par][:], F6[0][:], F5[:], ALU.mult, ["F6_0", "F5"], ["qdT%d" % par])
            tt("pool", kiT[par][:], F2[:], F3[:], ALU.mult, ["F2", "F3"], ["kiT%d" % par])
            yield
            bi, bik = inproj_tok(2048)
            cp("act", v_tok[par][:], bi[:], [bik], ["v_tok%d" % par])
            yield
            for h in range(4):
                for c in range(2):
                    lo = h * 128 + c * 64
                    stt(keT[par][:, lo:lo + 64], F2[:, lo:lo + 64], F5[:, lo + 63:lo + 64], F3[:, lo:lo + 64], ALU.mult, ALU.mult,
                        ["F2", "F5", "F3"], ["keT%d" % par])
            cp("pool", dec[:, par * 8:(par + 1) * 8], F5[:, 63:512:64], ["F5"], ["dec%d" % par])
            yield
            bo, bok = inproj_tok(2560)
            act(sog[par][:], bo[:], AF.Sigmoid, [bok], ["sog%d" % par])
            yield
            for j in range(2):
                bg, bgk = inproj_tok(3072 + j * 512)
                act(sga[par][:, j * 512:(j + 1) * 512], bg[:], AF.Sigmoid, [bgk], ["sga%d" % par])
                yield
            for j in range(2):
                bg, bgk = inproj_tok(4096 + j * 512)
                act(sgb[par][:, j * 512:(j + 1) * 512], bg[:], AF.Sigmoid, [bgk], ["sgb%d" % par])
                yield

        gdone = {}

        def back_g(t):
            par = t % 2
            bs_, bsk = nextM()
            for g in range(8):
                gs = slice(g * 64, (g + 1) * 64)
                mm(bs_[:, gs], wspT[:, g, :], vn[par][:, gs], True, True, ["wspT", "vn%d" % par], [bsk])
            for g in range(8):
                gs = slice(g * 64, (g + 1) * 64)
                stt(ya[:, gs], bs_[:, gs], bsp[:, g:g + 1], gu[par][:, gs], ALU.add, ALU.mult, [bsk, "bsp", "gu%d" % par], ["ya"])
            yield
            T4g = T8[:, 4:8, :]
            transpose8(ya, "ya", T4g, "T8", n=4)
            yield
            banksA = []
            for half in range(2):
                bk_, bkk = nextM()
                for c in range(4):
                    mm(bk_[:], T4g[:, c, :], wua[:, c, half * 512:(half + 1) * 512], c == 0, c == 3, ["T8", "wua"], [bkk])
                banksA.append((bk_, bkk))
            for half in range(2):
                bk_, bkk = banksA[half]
                tt("dve", bigf2[:, half * 512:(half + 1) * 512], bk_[:], sga[par][:, half * 512:(half + 1) * 512], ALU.mult, [bkk, "sga%d" % par], ["bigf2"])
            yield
            gdone[t] = True
            yield

        def back(t):
            par = t % 2
            xb_ = xt[par]
            xk = "xt%d" % par
            qd, qdk = qdT[par], "qdT%d" % par
            ki, kik = kiT[par], "kiT%d" % par
            ke, kek = keT[par], "keT%d" % par
            vtk, vtkk = v_tok[par], "v_tok%d" % par
            if t % TPS == 0:
                memset("pool", Sst[:], 0.0, ["Sst"])
                memset("pool", Sbf[:], 0.0, ["Sbf"])
            for h in range(4):
                tr(pT[:, h * 128:(h + 1) * 128], ke[:, h * 128:(h + 1) * 128], ident_b[:], [kek, "ident_b"], ["pT"])
            cp("act", ke_tok[:], pT[:, 0:512], ["pT"], ["ke_tok"])
            for h in range(4):
                hs = slice(h * 128, (h + 1) * 128)
                mm(pA[:, hs], ki[:, hs], qd[:, hs], True, True, [kik, qdk], ["pA"])
            tt("dve", attT[:], pA[:], hmask4[:], ALU.mult, ["pA", "hmask4"], ["attT"])
            yield
            if t >= 1:
                rank_step(t - 1)
            for h in range(4):
                hs = slice(h * 128, (h + 1) * 128)
                mm(pD[:, hs], ke_tok[0:64, hs], vtk[0:64, hs], True, True, ["ke_tok", vtkk], ["pD"])
            for h in range(4):
                hs = slice(h * 128, (h + 1) * 128)
                mm(pO[:, hs], attT[:, hs], vtk[:, hs], h == 0, False, ["attT", vtkk], ["pO"], skip_group_check=True)
                mm(pO[0:64, hs], qd[:, h * 128:h * 128 + 64], Sbf[:, hs], False, False, [qdk, "Sbf"], ["pO"], skip_group_check=True)
            for h in range(4):
                hs = slice(h * 128, (h + 1) * 128)
                stt(Sst[:, hs], Sst[:, hs], dec[:, par * 8 + 2 * h:par * 8 + 2 * h + 1], pD[:, hs], ALU.mult, ALU.add, ["Sst", "dec%d" % par, "pD"], ["Sst"])
            cp("pool", Sbf[:], Sst[:], ["Sst"], ["Sbf"])
            yield
            for h in range(4):
                hs = slice(h * 128, (h + 1) * 128)
                mm(pD[:, hs], ke_tok[64:128, hs], vtk[64:128, hs], True, True, ["ke_tok", vtkk], ["pD"])
            for h in range(4):
                hs = slice(h * 128, (h + 1) * 128)
                mm(pO[64:128, hs], qd[:, h * 128 + 64:h * 128 + 128], Sbf[:, hs], False, True, [qdk, "Sbf"], ["pO"], skip_group_check=True)
            for h in range(4):
                hs = slice(h * 128, (h + 1) * 128)
                stt(Sst[:, hs], Sst[:, hs], dec[:, par * 8 + 2 * h + 1:par * 8 + 2 * h + 2], pD[:, hs], ALU.mult, ALU.add, ["Sst", "dec%d" % par, "pD"], ["Sst"])
            cp("pool", Sbf[:], Sst[:], ["Sst"], ["Sbf"])
            yield
            for h in range(4):
                hs = slice(h * 128, (h + 1) * 128)
                act(tmpB[:, hs], pO[:, hs], AF.Square, ["pO"], ["m2h", "sm8"], accum_out=sm[:, 8 + h:9 + h])
            ts("pool", sm[:, 8:12], sm[:, 8:12], 1.0 / 128, EPS, ALU.mult, ALU.add, ["sm8"], ["sm8"])
            tt("pool", sm[:, 8:12], sm[:, 8:12], nhalf[:, 0:4], ALU.pow, ["sm8", "nhalf"], ["sm8"])
            for h in range(4):
                hs = slice(h * 128, (h + 1) * 128)
                stt(tmpB[:, hs], pO[:, hs], sm[:, 8 + h:9 + h], ng_bc[:, hs], ALU.mult, ALU.mult, ["pO", "sm8", "ng_bc"], ["m2h"])
            tt("pool", yb[:], tmpB[:], sog[par][:], ALU.mult, ["m2h", "sog%d" % par], ["yb"])
            yield
            yield
            assert gdone.get(t)
            transpose8(yb, "yb", T4, "T4", n=4)
            yield
            for half in range(2):
                hs = slice(half * 512, (half + 1) * 512)
                bk_, bkk = nextM()
                for c in range(4):
                    mm(bk_[:], T4[:, c, :], wub[:, c, hs], c == 0, c == 3, ["T4", "wub"], [bkk])
                tt("dve", m2h[:], bk_[:], sgb[par][:, hs], ALU.mult, [bkk, "sgb%d" % par], ["m2h"])
                tt("dve", B8[:, hs], m2h[:], bigf2[:, hs], ALU.add, ["m2h", "bigf2"], ["B8"])
            yield
            yield
            transpose8(B8, "B8", T8, "T8")
            yield
            for half in range(2):
                hs = slice(half * 512, (half + 1) * 512)
                bk_, bkk = nextM()
                for kc in range(8):
                    mm(bk_[:], T8[:, kc, :], wo[:, kc, hs], kc == 0, kc == 7, ["T8", "wo"], [bkk])
                tt("dve", xb_[:, hs], bk_[:], xb_[:, hs], ALU.add, [bkk, xk], [xk])
            dma("sp", x1_d[t * 128:(t + 1) * 128, :], xb_[:], "x1st%d" % (t % 2), [xk], ["x1_d"])
            if dbg:
                dma("sp", dbg_d["x1"][t * 128:(t + 1) * 128, :], xb_[:], "dbgx1", [xk], [])
            yield
            rmsnorm_to_bf16(xb_, xk, gbc1, "gbc1", B8, "B8", 1)
            dma("sp", h2_d[t * 128:(t + 1) * 128, :], B8[:], "h2st", ["B8"], ["h2_d"])
            yield
            yield
            transpose8(B8, "B8", T8, "T8")
            yield
            for kc in range(8):
                mm(pA[:, 0:36], T8[:, kc, :], wr[:, kc, :], kc == 0, kc == 7, ["T8", "wr0", "wr1"], ["pA"])
            cp("dve", lg[:], pA[:, 0:36], ["pA"], ["lg"])
            yield
            cp("pool", gla[:, t, :], lg[:, 0:4], ["lg"], ["gla"])
            cp("dve", rt[:, 40:44], lg[:, 0:4], ["lg"], ["rt"])
            A("dve", lambda e: e.max(out=rt[:, 48:56], in_=rt[:, 40:48]), reads=["rt"], writes=["rt"])
            A("dve", lambda e: e.max_index(out=ridx[:, 0:8], in_max=rt[:, 48:56], in_values=rt[:, 40:48]), reads=["rt"], writes=["ridx"])
            cp("dve", gmxa[:, t:t + 1], rt[:, 48:49], ["rt"], ["gmxa"])
            cp("dve", rt[:, 56:57], ridx[:, 0:1], ["ridx"], ["rt"])
            ts("dve", rt[:, 0:4], io8[:, 0:4], rt[:, 56:57], None, ALU.is_equal, None, ["io8", "rt"], ["rt"])
            ts("dve", rt[:, 8:16], lg[:, 4:12], rt[:, 0:1], None, ALU.mult, None, ["lg", "rt"], ["rt"])
            for g in range(1, 4):
                stt(rt[:, 8:16], lg[:, 4 + g * 8:12 + g * 8], rt[:, g:g + 1], rt[:, 8:16], ALU.mult, ALU.add, ["lg", "rt"], ["rt"])
            A("dve", lambda e: e.max(out=rt[:, 16:24], in_=rt[:, 8:16]), reads=["rt"], writes=["rt"])
            A("dve", lambda e: e.max_index(out=ridx[:, 8:16], in_max=rt[:, 16:24], in_values=rt[:, 8:16]), reads=["rt"], writes=["ridx"])
            tt("dve", d12a[:, t:t + 1], rt[:, 16:17], rt[:, 17:18], ALU.subtract, ["rt"], ["d12a"])
            cp("dve", rt[:, 58:60], ridx[:, 8:10], ["ridx"], ["rt"])
            ts("dve", rt[:, 24:32], io8[:], rt[:, 58:59], None, ALU.is_equal, None, ["io8", "rt"], ["rt"])
            ts("dve", rt[:, 32:40], io8[:], rt[:, 59:60], None, ALU.is_equal, None, ["io8", "rt"], ["rt"])
            for g in range(4):
                ts("dve", M1a[:, t, g * 8:(g + 1) * 8], rt[:, 24:32], rt[:, g:g + 1], None, ALU.mult, None, ["rt"], ["M1a"])
                ts("pool", M2a[:, t, g * 8:(g + 1) * 8], rt[:, 32:40], rt[:, g:g + 1], None, ALU.mult, None, ["rt"], ["M2a"])
            tt("pool", Msum[par][:], M1a[:, t, :], M2a[:, t, :], ALU.add, ["M1a", "M2a"], ["Msum%d" % par])
            yield

        def rank_step(t):
            par = t % 2
            mm(pA[:, 64:96], stri_b[:], Msum[par][:], True, True, ["stri_b", "Msum%d" % par], ["pA"])
            mm(pA[:, 96:128], ones_b[:], Msum[par][:], True, True, ["ones_b", "Msum%d" % par], ["pA"])
            tt("dve", rka[:, t, :], pA[:, 64:96], runtot[:], ALU.add, ["pA", "runtot"], ["rka"])
            tt("dve", runtot[:], pA[:, 96:128], runtot[:], ALU.add, ["pA", "runtot"], ["runtot"])

        def interleave(gens):
            gens = [g for g in gens if g is not None]
            while gens:
                for g in list(gens):
                    try:
                        next(g)
                    except StopIteration:
                        gens.remove(g)

        if "A" in phases:
            interleave([front(0)])
            nz = NBLK
            zdone = 0
            for t in range(nt):
                for _ in range(2):
                    if zdone < nz and t >= 1:
                        zero_ops.append(dma("sp", xbuf_d[zdone * BR:(zdone + 1) * BR, :], zsrc_d[0:BR, :], "xzero", ["zsrc_d"], []))
                        zdone += 1
                fr_ = front(t + 1) if t + 1 < nt else None
                if KV == 1:
                    interleave([back(t), fr_, back_g(t)])
                elif KV == 2:
                    interleave([fr_, back(t), back_g(t)])
                else:
                    interleave([fr_, back_g(t), back(t)])
                if MODE == 1 and t + 1 < nt:
                    interleave([front(t + 1)])

        if "A" in phases:
            rank_step(nt - 1)
            while zdone < nz:
                zero_ops.append(dma("sp", xbuf_d[zdone * BR:(zdone + 1) * BR, :], zsrc_d[0:BR, :], "xzero", ["zsrc_d"], []))
                zdone += 1
        E1 = [0, 12288, 24576]
        OFF_PG = 0
        OFF_PLE = 8192
        if "B" in phases:
            lastA = [last_inproj[0]]
            BF3 = ["bigf3", "qdT1", "kiT1", "gv"]
            dma("pool", gbc0[:], g_ple_d.partition_broadcast(128), "gbc0", [], ["gbc0"])
            dma("sp", F1[0][:], g_final_d[:, 0:512].partition_broadcast(128), "gfin0", [], ["F1_0"])
            dma("sp", F2[:], g_final_d[:, 512:1024].partition_broadcast(128), "gfin1", [], ["F2"])
            tot = pb[:, 0, :]
            pad = pb[:, 1, :]
            pend = pb[:, 2, :]
            pstart = pb[:, 3, :]
            tmp32 = pb[:, 4, :]
            ones32 = pb[:, 5, :]
            memset("dve", ones32, 1.0, ["pb5"])
            cmp3 = bigf3[:].rearrange("p (e j) -> p e j", e=32)
            tt("dve", cmp3, runtot[:, :, None].to_broadcast([128, 32, 32]), Jt[:, None, 0:32].to_broadcast([128, 32, 32]),
               ALU.is_gt, ["runtot", "Jt"], BF3)
            A("dve", lambda e: e.tensor_reduce(out=pad, in_=cmp3, axis=AX.X, op=ALU.add), reads=["bigf3"], writes=["pb1"])
            ts("dve", pad, pad, float(BR), None, ALU.mult, None, ["pb1"], ["pb1"])
            A("dve", lambda e: e.tensor_tensor_scan(out=pend, data0=ones32, data1=pad, initial=0.0, op0=ALU.mult, op1=ALU.add),
              reads=["pb5", "pb1"], writes=["pb2"])
            tt("dve", pstart, pend, pad, ALU.subtract, ["pb2", "pb1"], ["pb3"])
            for bp in range(2):
                tt("dve", cmp3, pend[:, None, :].to_broadcast([128, 32, 32]), Jt[:, bp * 32:(bp + 1) * 32, None].to_broadcast([128, 32, 32]),
                   ALU.is_le, ["pb2", "Jt"], BF3)
                A("dve", lambda e, bp=bp: e.tensor_reduce(out=blkrow[:, bp * 32:(bp + 1) * 32], in_=cmp3, axis=AX.X, op=ALU.add),
                  reads=["bigf3"], writes=["blkrow"])
            ts("dve", blkrow[:], blkrow[:], 31.0, 128.0, ALU.min, ALU.mult, ["blkrow"], ["blkrow"])
            ts("dve", blkrow[:], blkrow[:], pid[:, 0:1], None, ALU.add, None, ["blkrow", "pid"], ["blkrow"])
            unu = bigf2[:, 0:96]
            ts("dve", unu, Jt[:, 0:96], pb[:, 2, 31:32], None, ALU.is_ge, None, ["Jt", "pb2"], ["bigf2"])
            ts("dve", unu, unu, pmask[:, 0:1], 4096.0, ALU.mult, ALU.mult, ["bigf2", "pmask"], ["bigf2"])
            tt("dve", blkrow[:, SKIP0:64], blkrow[:, SKIP0:64], unu[:, SKIP0:64], ALU.add, ["blkrow", "bigf2"], ["blkrow"])
            cp("dve", idxall[:], blkrow[:], ["blkrow"], ["idxall"])
            ts("dve", tmp32, pend, bstart[:, 0:1], None, ALU.is_le, None, ["pb2", "bstart"], ["pb4"])
            A("dve", lambda e: e.tensor_reduce(out=metaf[:, 0:1], in_=tmp32, axis=AX.X, op=ALU.add), reads=["pb4"], writes=["metaf"])
            ts("dve", metaf[:, 0:1], metaf[:, 0:1], 31.0, None, ALU.min, None, ["metaf"], ["metaf"])
            ts("dve", metaf[:, 1:2], pb[:, 2, 31:32], bstart[:, 0:1], None, ALU.is_gt, None, ["pb2", "bstart"], ["metaf"])
            cp("dve", metai[:], metaf[:], ["metaf"], ["metai"])
            dma("sp", meta_d, metai[:], "meta", ["metai"], ["meta_d"])
            if dbg:
                dma("sp", dbg_d["meta"], metai[:], "dbgm", ["metai"], [])
            for t in range(nt):
                tt("dve", tmp32, rka[:, t, :], pstart, ALU.add, ["rka", "pb3"], ["pb4"])
                stt(pb[:, 5, :], M1a[:, t, :], 1.0, tmp32, ALU.mult, ALU.mult, ["M1a", "pb4"], ["pb5", "d1f"], accum=d1f[:, t:t + 1])
                stt(pb[:, 5, :], M2a[:, t, :], 1.0, tmp32, ALU.mult, ALU.mult, ["M2a", "pb4"], ["pb5", "d2f"], accum=d2f[:, t:t + 1])
            cp("dve", dsti[:, 0:nt], d1f[:, 0:nt], ["d1f"], ["dsti"])
            cp("dve", dsti[:, NT:NT + nt], d2f[:, 0:nt], ["d2f"], ["dsti"])
            if dbg:
                dma("sp", dbg_d["dest"], dsti[:], "dbgd", ["dsti"], [])
            tt("dve", gla[:], gla[:], gmxa[:, :, None].to_broadcast([128, NT, 4]), ALU.subtract, ["gla", "gmxa"], ["gla"])
            act(gla[:], gla[:], AF.Exp, ["gla"], ["gla"])
            A("dve", lambda e: e.tensor_reduce(out=gmxa[:], in_=gla[:], axis=AX.X, op=ALU.add), reads=["gla"], writes=["gmxa"])
            A("dve", lambda e: e.reciprocal(out=gmxa[:], in_=gmxa[:]), reads=["gmxa"], writes=["gmxa"])
            act(d12a[:], d12a[:], AF.Sigmoid, ["d12a"], ["d12a"])
            tt("dve", g1a[:], d12a[:], gmxa[:], ALU.mult, ["d12a", "gmxa"], ["g1a"])
            tt("dve", g2a[:], gmxa[:], g1a[:], ALU.subtract, ["gmxa", "g1a"], ["g2a"])
            if dbg:
                dma("sp", dbg_d["gate"][:, 0:NT], g1a[:], "dbgg", ["g1a"], [])
                dma("sp", dbg_d["gate"][:, NT:2 * NT], g2a[:], "dbgg", ["g2a"], [])
            def emit_wload(b):
                j = b % 3
                base = E1[j]

                def wload(e, b=b, base=base):
                    off = bass.IndirectOffsetOnAxis(ap=idxall[:, b:b + 1], axis=0)
                    kw = dict(bounds_check=4095, oob_is_err=False) if b >= SKIP0 else {}
                    i0 = e.indirect_dma_start(out=wbig[:, base:base + 4096], out_offset=None,
                                              in_=w1_d[0].rearrange("e (p j) n -> (e p) (j n)", j=8), in_offset=off, **kw)
                    i1 = e.indirect_dma_start(out=wbig[:, base + 4096:base + 8192], out_offset=None,
                                              in_=w3_d[0].rearrange("e (p j) n -> (e p) (j n)", j=8), in_offset=off, **kw)
                    i2 = e.indirect_dma_start(out=wbig[:, base + 8192:base + 12288], out_offset=None,
                                              in_=w2_d[0].rearrange("e (p j) n -> (e p) (j n)", j=4), in_offset=off, **kw)
                    return [i0, i1, i2]
                op = A("pool", wload, reads=["idxall"], writes=["we_%d" % j], dma="we_%d" % j, n=3)
                if b < 3 and last_inproj[0] is not None:
                    op.deps.add(last_inproj[0]); last_inproj[0].has_dep = True
            for b0 in range(min(3, nblk)):
                emit_wload(b0)
            for t in range(nt):
                hb = B8 if t % 2 == 0 else B8b
                hk = "B8" if t % 2 == 0 else "B8b"
                dma("sp", hb[:], h2_d[t * 128:(t + 1) * 128, :], "h2ld%d" % (t % 2), ["h2_d"], [hk])
                for k in range(2):
                    col = k * NT + t
                    A("pool", lambda e, col=col, hb=hb: e.indirect_dma_start(
                        out=xbuf_d, out_offset=bass.IndirectOffsetOnAxis(ap=dsti[:, col:col + 1], axis=0), in_=hb[:], in_offset=None),
                      reads=[hk, "dsti"], writes=["xbuf_w%d_%d" % (k, t % 2)], dma="scat%d_%d" % (k, t % 2))
                    if t == 0:
                        S.ops[-1].deps.update(zero_ops)

        if "C" in phases:
            scat_ops = [op for op in S.ops if op.dma is not None and op.dma.startswith("scat")]
            NSB = BR // 128
            ABK = [((pM[0], "pM0"), (pM[1], "pM1")), ((pQ, "pQ"), (pF, "pF"))]

            XB3 = [(B8, "B8"), (B8b, "B8b"), (sga[0], "sga0")]

            def c0(q):
                b, sbi = divmod(q, NSB)
                r0 = b * BR + sbi * 128
                xb_sb, xbk = XB3[q % 3]
                op = dma("sp", xb_sb[:], xbuf_d[r0:r0 + 128, :], "xbld%d" % (q % 3), [], [xbk])
                if q < 3:
                    for so in scat_ops:
                        op.deps.add(so)

            def c1(q):
                b, sbi = divmod(q, NSB)
                j = b % 3
                base = E1[j]
                qp = q % 2
                r0 = b * BR + sbi * 128
                xb_sb, xbk = XB3[q % 3]
                T8q, T8qk = (T8, "T8") if qp == 0 else (T8f, "T8f")
                transpose8(xb_sb, xbk, T8q, T8qk, stride=8)
                (bA, bAk), (bB, bBk) = ABK[qp]
                for kc in range(8):
                    mm(bA[:], T8q[:, kc, :], wbig[:, base + kc * 512: base + (kc + 1) * 512], kc == 0, kc == 7, [T8qk, "we_%d" % j], [bAk])
                for kc in range(8):
                    mm(bB[:], T8q[:, kc, :], wbig[:, base + 4096 + kc * 512: base + 4096 + (kc + 1) * 512], kc == 0, kc == 7, [T8qk, "we_%d" % j], [bBk])

            def c2(q):
                b, sbi = divmod(q, NSB)
                j = b % 3
                base = E1[j]
                qp = q % 2
                r0 = b * BR + sbi * 128
                (bA, bAk), (bB, bBk) = ABK[qp]
                act(tmpA[:], bA[:], AF.Sigmoid, [bAk], ["tmpA"])
                tt("dve", tmpA[:], tmpA[:], bA[:], ALU.mult, ["tmpA", bAk], ["tmpA"])
                yq, yqk = (ya, "ya") if qp == 0 else (yb, "yb")
                tt("dve", yq[:], tmpA[:], bB[:], ALU.mult, ["tmpA", bBk], [yqk])
                transpose8(yq, yqk, T4, "T4", n=4, stride=4)
                ysb = xt[qp][:, 0:512].bitcast(BF16)
                yk = "xt%d" % qp
                for half, (bY, bYk) in enumerate(((pO, "pO"), (pD, "pD"))):
                    for c in range(4):
                        mm(bY[:], T4[:, c, :], wbig[:, base + 8192 + c * 1024 + half * 512: base + 8192 + c * 1024 + (half + 1) * 512],
                           c == 0, c == 3, ["T4", "we_%d" % j], [bYk])
                    cp("act" if half == 0 else "dve", ysb[:, half * 512:(half + 1) * 512], bY[:], [bYk], [yk])
                dma("sp", ybuf_d[r0:r0 + 128, :], ysb[:], "yst%d" % qp, [yk], ["ybuf_d"])

            nq = nblk * NSB
            c0(0)
            c0(1)
            c1(0)
            for q in range(nq):
                if q + 2 < nq:
                    c0(q + 2)
                if q + 1 < nq:
                    c1(q + 1)
                c2(q)
                if (q + 1) % NSB == 0:
                    nb_ = (q + 1) // NSB + 2
                    if nb_ < nblk:
                        emit_wload(nb_)

        if "D" in phases:
            dma("pool", wbig[:, OFF_PG:OFF_PG + 8192].rearrange("p (c n) -> p c n", c=8), w_pg_d[0].rearrange("(c p) n -> p c n", p=128),
                "wpg", [], ["wpg", "we_0"])
            dma("pool", wbig[:, OFF_PLE:OFF_PLE + 2048].rearrange("p (c n) -> p c n", c=2), w_ple_d[0].rearrange("(c p) n -> p c n", p=128),
                "wple", [], ["wple", "we_0"])
            yst_ops = [op for op in S.ops if op.dma is not None and op.dma.startswith("yst")]
            last_pe = next(op for op in reversed(S.ops) if op.eng == "pe")
            last_pe.has_dep = True
            BF3 = ["bigf3", "qdT1", "kiT1", "gv"]
            T8x = [T8, T8f]
            T8k = ["T8", "T8f"]
            WF = wbig[:, 12288:40960].bitcast(F32)
            xr3 = [xt[0][:], xt[1][:], WF[:, 0:1024], WF[:, 4096:5120]]
            xr3k = ["xt0", "xt1", "wf_x2", "wf_x3"]
            ygA = [bigf2[:, 0:512].bitcast(BF16), WF[:, 1024:1536].bitcast(BF16)]
            ygAk = [["bigf2"], ["wf_ya"]]
            ygB = [bigf3[:, 0:512].bitcast(BF16), WF[:, 1536:2048].bitcast(BF16)]
            ygBk = [BF3, ["wf_yb"]]
            ptl = [F3[:, 0:256], F3[:, 256:512]]
            ptlk = ["F3a", "F3b"]
            B8x = [B8, B8b]
            B8k = ["B8", "B8b"]
            ost = [(F4[:], "F4", F5[:], "F5"), (WF[:, 2048:2560], "wf_o0", WF[:, 2560:3072], "wf_o1")]
            junk = sga[0]
            B8x = [B8, B8b]
            first_wf = {}

            def wfdep(op, key):
                if key.startswith("wf_") and key not in first_wf:
                    first_wf[key] = True
                    op.deps.add(last_pe)
                return op

            def s1(t):
                i3 = t % 4
                par = t % 2
                xr, xk = xr3[i3], xr3k[i3]
                wfdep(dma("sp", xr, x1_d[t * 128:(t + 1) * 128, :], "x1ld%d" % i3, ["x1_d"], [xk]), xk)
                for k, (yb_, ybk) in enumerate(((ygA[par], ygAk[par]), (ygB[par], ygBk[par]))):
                    col = k * NT + t
                    og_ = A("pool", lambda e, col=col, yb_=yb_: e.indirect_dma_start(
                        out=yb_, out_offset=None, in_=ybuf_d, in_offset=bass.IndirectOffsetOnAxis(ap=dsti[:, col:col + 1], axis=0)),
                        reads=["dsti"], writes=ybk, dma="gath%d_%d" % (k, par))
                    wfdep(og_, ybk[0])
                    if t == 0:
                        for yo in yst_ops:
                            og_.deps.add(yo)
                dma("sp", ptl[par], p_d[t * 128:(t + 1) * 128, :], "pld%d" % par, [], [ptlk[par], "F3"])
                yield

            def s2(t):
                i3 = t % 4
                par = t % 2
                xr, xk = xr3[i3], xr3k[i3]
                stt(xr, ygA[par], g1a[:, t:t + 1], xr, ALU.mult, ALU.add, ygAk[par] + ["g1a", xk], [xk])
                stt(xr, ygB[par], g2a[:, t:t + 1], xr, ALU.mult, ALU.add, ygBk[par] + ["g2a", xk], [xk])
                if dbg:
                    dma("sp", dbg_d["x2"][t * 128:(t + 1) * 128, :], xr, "dbgx2", [xk], [])
                cp("act", ptbx[par], ptl[par], [ptlk[par]], ["ptb%d" % par])
                yield
                rmsnorm_to_bf16(xr, xk, gbc0, "gbc0", B8x[par], B8k[par], 2)
                yield

            def s2b(t):
                par = t % 2
                transpose8(B8x[par], B8k[par], T8x[par], T8k[par])
                yield
                for c in range(2):
                    tr(pT[:, c * 128:(c + 1) * 128], ptbx[par][:, c * 128:(c + 1) * 128], ident_b[:], ["ptb%d" % par, "ident_b"], ["pT"])
                cp("act", T4[:, 2 * par:2 * par + 2, :].rearrange("p c t -> p (c t)"), pT[:, 0:256], ["pT"], ["T4_%d" % par, "T4"])
                yield

            def s3(t):
                i3 = t % 4
                par = t % 2
                xr, xk = xr3[i3], xr3k[i3]
                for half in range(2):
                    hs = slice(half * 512, (half + 1) * 512)
                    bG, bGk = nextM()
                    for kc in range(8):
                        mm(bG[:], T8x[par][:, kc, :], wbig[:, OFF_PG + kc * 1024 + half * 512: OFF_PG + kc * 1024 + (half + 1) * 512],
                           kc == 0, kc == 7, [T8k[par], "wpg"], [bGk])
                    act(tmpA[:], bG[:], AF.Sigmoid, [bGk], ["tmpA"])
                    bP, bPk = (pO, "pO") if half == 0 else (pD, "pD")
                    for c in range(2):
                        mm(bP[:], T4[:, 2 * par + c, :], wbig[:, OFF_PLE + c * 1024 + half * 512: OFF_PLE + c * 1024 + (half + 1) * 512],
                           c == 0, c == 1, ["T4_%d" % par, "wple"], [bPk])
                    tt("dve", tmpA[:], tmpA[:], bP[:], ALU.mult, ["tmpA", bPk], ["tmpA"])
                    tt("dve", xr[:, hs], xr[:, hs], tmpA[:], ALU.add, [xk, "tmpA"], [xk])
                    yield
                act(junk[:], xr, AF.Square, [xk], ["sga0", "sm3"], accum_out=sm[:, 3:4])
                ts("pool", sm[:, 3:4], sm[:, 3:4], 1.0 / D, EPS, ALU.mult, ALU.add, ["sm3"], ["sm3"])
                tt("pool", sm[:, 3:4], sm[:, 3:4], nhalf[:, 0:1], ALU.pow, ["sm3", "nhalf"], ["sm3"])
                yield
                o0, o0k, o1, o1k = ost[par]
                wfdep(stt(o0, xr[:, 0:512], sm[:, 3:4], F1[0][:], ALU.mult, ALU.mult, [xk, "sm3", "F1_0"], [o0k]), o0k)
                wfdep(stt(o1, xr[:, 512:1024], sm[:, 3:4], F2[:], ALU.mult, ALU.mult, [xk, "sm3", "F2"], [o1k]), o1k)
                dma("sp", out_d[t * 128:(t + 1) * 128, 0:512], o0, "outst0_%d" % par, [o0k], ["out_d0"])
                dma("sp", out_d[t * 128:(t + 1) * 128, 512:1024], o1, "outst1_%d" % par, [o1k], ["out_d1"])
                yield

            ptbx = [ptb[:, 0:256], WF[:, 3072:3200].bitcast(BF16)]
            for step in range(nt + 3):
                interleave([s3(step - 3) if 0 <= step - 3 < nt else None,
                            s2b(step - 2) if 0 <= step - 2 < nt else None,
                            s2(step - 1) if 0 <= step - 1 < nt else None,
                            s1(step) if step < nt else None])
        S.emit(nc)
    return nc


_CACHE = {}


def kernel(**inputs):
    names = ["g_mix", "w_in", "gm_ln_g", "gm_ln_b", "gm_w_sp", "gm_b_sp", "w_up_a", "hg_lb_param", "hg_norm_g", "w_up_b",
             "w_out", "g_ffn", "w_grp", "w_exp", "w1", "w3", "w2", "g_ple", "w_pg", "w_ple"]
    x = np.ascontiguousarray(np.asarray(inputs["x"], dtype=np.float32)).reshape(NCORES, TOK, D)
    p = np.ascontiguousarray(np.asarray(inputs["p"], dtype=np.float32)).reshape(NCORES, TOK, 256)
    shared = {n: np.ascontiguousarray(np.asarray(inputs[n], dtype=np.float32)) for n in names}
    shared["g_final"] = np.ascontiguousarray(np.asarray(inputs["g_final"], dtype=np.float32)).reshape(1, D)
    if "nc" not in _CACHE:
        _CACHE["nc"] = build()
    nc = _CACHE["nc"]
    in_maps = []
    for c in range(NCORES):
        m = dict(shared)
        m["x"] = x[c]
        m["p"] = p[c]
        in_maps.append(m)
    res = run_bass_kernel_spmd(nc, in_maps, core_ids=list(range(NCORES)))
    out = np.stack([np.asarray(r["out"], dtype=np.float32) for r in res.results], axis=0)
    return out.reshape(16, 2048, D)
```

```python
import contextlib
import numpy as np
import concourse.bass as bass
import concourse.mybir as mybir
from concourse.bass_utils import run_bass_kernel_spmd

F32 = mybir.dt.float32
BF16 = mybir.dt.bfloat16
I32 = mybir.dt.int32
ALU = mybir.AluOpType
AF = mybir.ActivationFunctionType
AX = mybir.AxisListType

import os
MODE = 2
KV = int(os.environ.get('KV', '1'))
NCORES = 8
TOK = 4096
NT = TOK // 128
TPS = 16
D = 1024
NBLK = 64
BR = 256
SKIP0 = 51
EPS = 1e-6
EPOCH = 4000
ENGS = ("pe", "act", "dve", "pool", "sp")


class Op:
    __slots__ = ("eng", "fn", "deps", "has_dep", "dma", "sem", "val", "idx", "force", "n")

    def __init__(self, eng, fn, dma, idx):
        self.eng = eng
        self.fn = fn
        self.dma = dma
        self.deps = set()
        self.has_dep = False
        self.sem = None
        self.val = 0
        self.idx = idx
        self.force = False
        self.n = 1


class Sched:
    def __init__(self):
        self.ops = []
        self.last_w = {}
        self.readers = {}

    def add(self, eng, fn, reads=(), writes=(), dma=None, after=(), n=1):
        op = Op(eng, fn, dma, len(self.ops))
        op.n = n
        deps = set(a for a in after if a is not None)
        for k in reads:
            w = self.last_w.get(k)
            if w is not None:
                deps.add(w)
        for k in writes:
            w = self.last_w.get(k)
            if w is not None:
                deps.add(w)
            for r in self.readers.get(k, ()):
                deps.add(r)
        for k in reads:
            self.readers.setdefault(k, []).append(op)
        for k in writes:
            self.last_w[k] = op
            self.readers[k] = []
        deps.discard(op)
        if eng == "pe":
            deps = set(d for d in deps if d.eng != "pe" or d.dma is not None)
        op.deps = deps
        for d in deps:
            d.has_dep = True
        self.ops.append(op)
        return op

    def emit(self, nc, final_wait_eng="sp"):
        cnt = {e: 0 for e in ENGS}
        dma_cnt = {}
        sem_names = set()
        for op in self.ops:
            if op.dma is not None:
                dma_cnt[op.dma] = dma_cnt.get(op.dma, 0) + op.n
                op.sem = "dma_" + op.dma
                op.val = 16 * dma_cnt[op.dma]
                sem_names.add(op.sem)
            elif op.has_dep:
                c = cnt[op.eng]
                cnt[op.eng] = c + 1
                op.sem = "c_%s_%d" % (op.eng, c // EPOCH)
                op.val = (c % EPOCH) + 1
                sem_names.add(op.sem)
        sem_names = sorted(sem_names)
        self.n_sems = len(sem_names)
        with contextlib.ExitStack() as st:
            sems = {n: st.enter_context(nc.semaphore(n)) for n in sem_names}
            block = st.enter_context(nc.Block())
            final = {}
            for op in self.ops:
                if op.dma is not None:
                    final[op.sem] = max(final.get(op.sem, 0), op.val)

            def run(engname, eng):
                waited = {}
                for op in self.ops:
                    if op.eng != engname:
                        continue
                    need = {}
                    for d in op.deps:
                        if waited.get(d.sem, 0) < d.val:
                            need[d.sem] = max(need.get(d.sem, 0), d.val)
                    for s, v in need.items():
                        eng.wait_ge(sems[s], v)
                        waited[s] = v
                    inst = op.fn(eng)
                    if op.sem is not None:
                        insts = inst if isinstance(inst, (list, tuple)) else [inst]
                        for ii in insts:
                            ii.then_inc(sems[op.sem], 16 if op.dma is not None else 1)
                if engname == final_wait_eng:
                    for s, v in final.items():
                        if waited.get(s, 0) < v:
                            eng.wait_ge(sems[s], v)

            @block.tensor
            def _(e):
                run("pe", e)

            @block.scalar
            def _(e):
                run("act", e)

            @block.vector
            def _(e):
                run("dve", e)

            @block.gpsimd
            def _(e):
                run("pool", e)

            @block.sync
            def _(e):
                run("sp", e)


def build(dbg=False, nt=NT, nblk=NBLK, phases="ABCD"):
    nc = bass.Bass("TRN2", target_bir_lowering=False)
    S = Sched()
    st = contextlib.ExitStack()

    def din(name, shape, dt=F32):
        return nc.dram_tensor(name, list(shape), dt, kind="ExternalInput").ap()

    x_d = din("x", [TOK, D])
    p_d = din("p", [TOK, 256])
    g_mix_d = din("g_mix", [1, D])
    w_in_d = din("w_in", [1, D, 5120])
    ln_g_d = din("gm_ln_g", [1, 512])
    ln_b_d = din("gm_ln_b", [1, 512])
    w_sp_d = din("gm_w_sp", [1, 8, 128, 128])
    b_sp_d = din("gm_b_sp", [1, 8, 128])
    w_up_a_d = din("w_up_a", [1, 512, D])
    lbp_d = din("hg_lb_param", [2, 512])
    ng_d = din("hg_norm_g", [1, 512])
    w_up_b_d = din("w_up_b", [1, 512, D])
    w_out_d = din("w_out", [1, D, D])
    g_ffn_d = din("g_ffn", [1, D])
    w_grp_d = din("w_grp", [1, D, 4])
    w_exp_d = din("w_exp", [1, D, 32])
    w1_d = din("w1", [1, 32, D, 512])
    w3_d = din("w3", [1, 32, D, 512])
    w2_d = din("w2", [1, 32, 512, D])
    g_ple_d = din("g_ple", [1, D])
    w_pg_d = din("w_pg", [1, D, D])
    w_ple_d = din("w_ple", [1, 256, D])
    g_final_d = din("g_final", [1, D])
    out_d = nc.dram_tensor("out", [TOK, D], F32, kind="ExternalOutput").ap()
    x1_d = nc.dram_tensor("x1_scr", [TOK, D], F32, kind="Internal").ap()
    h2_d = nc.dram_tensor("h2_scr", [TOK, D], BF16, kind="Internal").ap()
    xbuf_d = nc.dram_tensor("xbuf_scr", [NBLK * BR, D], BF16, kind="Internal").ap()
    ybuf_d = nc.dram_tensor("ybuf_scr", [NBLK * BR, D], BF16, kind="Internal").ap()
    meta_d = nc.dram_tensor("meta_scr", [128, 2], I32, kind="Internal").ap()
    zsrc_d = nc.dram_tensor("zsrc_scr", [512, D], BF16, kind="Internal").ap()
    dbg_d = {}
    if dbg:
        dbg_d["x1"] = nc.dram_tensor("dbg_x1", [TOK, D], F32, kind="ExternalOutput").ap()
        dbg_d["meta"] = nc.dram_tensor("dbg_meta", [128, 2], I32, kind="ExternalOutput").ap()
        dbg_d["dest"] = nc.dram_tensor("dbg_dest", [128, 64], I32, kind="ExternalOutput").ap()
        dbg_d["gate"] = nc.dram_tensor("dbg_gate", [128, 64], F32, kind="ExternalOutput").ap()
        dbg_d["x2"] = nc.dram_tensor("dbg_x2", [TOK, D], F32, kind="ExternalOutput").ap()

    def sb(name, shape, dt):
        return st.enter_context(nc.sbuf_tensor(name, list(shape), dt))

    def ps(name, shape, dt):
        return st.enter_context(nc.psum_tensor(name, list(shape), dt))

    wbig = sb("wbig", [128, 40960], BF16)
    wua = sb("wua", [128, 4, D], BF16)
    wub = sb("wub", [128, 4, D], BF16)
    wo = sb("wo", [128, 8, D], BF16)
    ident_b = sb("ident_b", [128, 128], BF16)
    ident_f = sb("ident_f", [128, 128], F32)
    ones_f = sb("ones_f", [128, 128], F32)
    ones_b = sb("ones_b", [128, 128], BF16)
    stri_b = sb("stri_b", [128, 128], BF16)
    tri_f = sb("tri_f", [128, 128], F32)
    hmask4 = sb("hmask4", [128, 512], BF16)
    wspT = sb("wspT", [128, 8, 128], BF16)
    bsp = sb("bsp", [128, 8], F32)
    lng_bc = sb("lng_bc", [128, 512], BF16)
    lnb_bc = sb("lnb_bc", [128, 512], BF16)
    ng_bc = sb("ng_bc", [128, 512], BF16)
    gbc0 = sb("gbc0", [128, D], BF16)
    gbc1 = sb("gbc1", [128, D], BF16)
    wr = sb("wr", [128, 8, 36], BF16)
    rmask = sb("rmask", [128, 512], BF16)
    pmask = sb("pmask", [128, 1], F32)
    lbp = sb("lbp", [128, 2, 4], F32)
    lb = sb("lb", [128, 4], F32)
    oml = sb("oml", [128, 4], F32)
    nhalf = sb("nhalf", [128, 4], F32)
    bstart = sb("bstart", [128, 1], F32)
    Jt = sb("Jt", [128, 96], F32)
    pid = sb("pid", [128, 1], F32)
    blkrow = sb("blkrow", [128, 96], F32)
    idxall = sb("idxall", [128, 96], I32)

    xt = [sb("xt0", [128, D], F32), sb("xt1", [128, D], F32)]
    bigf2 = sb("bigf2", [128, D], F32)
    bigf3 = sb("bigf3", [128, D], F32)
    B8 = sb("B8", [128, D], BF16)
    B8b = sb("B8b", [128, D], BF16)
    T8 = sb("T8", [128, 8, 128], BF16)
    T8f = sb("T8f", [128, 8, 128], BF16)
    T4 = sb("T4", [128, 4, 128], BF16)
    tmpA = sb("tmpA", [128, 512], F32)
    gu = [sb("gu0", [128, 512], BF16), sb("gu1", [128, 512], BF16)]
    gv = bigf3[:, 512:1024]
    vn = [sb("vn0", [128, 512], BF16), sb("vn1", [128, 512], BF16)]
    ya = sb("ya", [128, 512], BF16)
    v_tok = [sb("v_tok0", [128, 512], BF16), sb("v_tok1", [128, 512], BF16)]
    sog = [sb("sog0", [128, 512], BF16), sb("sog1", [128, 512], BF16)]
    sga = [sb("sga0", [128, D], BF16), sb("sga1", [128, D], BF16)]
    sgb = [sb("sgb0", [128, D], BF16), sb("sgb1", [128, D], BF16)]
    F1 = [sb("F1_0", [128, 512], F32)]
    F2 = sb("F2", [128, 512], F32)
    F3 = sb("F3", [128, 512], F32)
    F4 = sb("F4", [128, 512], F32)
    F5 = sb("F5", [128, 512], F32)
    F6 = [sb("F6_0", [128, 512], BF16), sb("F6_1", [128, 512], BF16)]
    _al = bigf3[:, 0:512].bitcast(BF16)
    qdT = [sb("qdT0", [128, 512], BF16), _al[:, 0:512]]
    kiT = [sb("kiT0", [128, 512], BF16), _al[:, 512:1024]]
    keT = [sb("keT0", [128, 512], BF16), F6[1]]
    dec = sb("dec", [128, 16], F32)
    ke_tok = sb("ke_tok", [128, 512], BF16)
    attT = sb("attT", [128, 512], BF16)
    Sst = sb("Sst", [128, 512], F32)
    Sbf = sb("Sbf", [128, 512], BF16)
    yb = sb("yb", [128, 512], BF16)
    m2h = sb("m2h", [128, 512], F32)
    tmpB = m2h
    st6 = sb("st6", [128, 6], F32)
    sm = sb("sm", [128, 32], F32)
    lg = sb("lg", [128, 36], F32)
    rt = sb("rt", [128, 64], F32)
    ridx = sb("ridx", [128, 16], mybir.dt.uint32)
    io8 = sb("io8", [128, 8], F32)
    M1a = sb("M1a", [128, NT, 32], BF16)
    M2a = sb("M2a", [128, NT, 32], BF16)
    Msum = [sb("Msum0", [128, 32], BF16), sb("Msum1", [128, 32], BF16)]
    rka = sb("rka", [128, NT, 32], F32)
    gla = sb("gla", [128, NT, 4], F32)
    gmxa = sb("gmxa", [128, NT], F32)
    d12a = sb("d12a", [128, NT], F32)
    g1a = sb("g1a", [128, NT], F32)
    g2a = sb("g2a", [128, NT], F32)
    runtot = sb("runtot", [128, 32], F32)
    pb = sb("pb", [128, 6, 32], F32)
    pbi = sb("pbi", [128, 32], I32)
    d1f = sb("d1f", [128, NT], F32)
    d2f = sb("d2f", [128, NT], F32)
    dsti = sb("dsti", [128, 2 * NT], I32)
    metaf = sb("metaf", [128, 2], F32)
    metai = sb("metai", [128, 2], I32)
    ptile = F3[:, 0:256]
    ptb = sb("ptb", [128, 256], BF16)

    pT = ps("pT", [128, 1024], BF16)
    pQ = ps("pQ", [128, 512], F32)
    pF = ps("pF", [128, 512], F32)
    pM = [ps("pM0", [128, 512], F32), ps("pM1", [128, 512], F32)]
    pO = ps("pO", [128, 512], F32)
    pD = ps("pD", [128, 512], F32)
    pA = ps("pA", [128, 512], F32)
    mrot = [0]

    def nextM():
        i = mrot[0] % 2
        mrot[0] += 1
        return pM[i], "pM%d" % i

    A = S.add

    def mm(out, lhsT, rhs, start, stop, reads, writes, **kw):
        return A("pe", lambda e: e.matmul(out=out, lhsT=lhsT, rhs=rhs, start=start, stop=stop, **kw), reads=reads, writes=writes)

    def tr(out, in_, ident, reads, writes):
        return A("pe", lambda e: e.transpose(out=out, in_=in_, identity=ident), reads=reads, writes=writes)

    def act(out, in_, func, reads, writes, **kw):
        return A("act", lambda e: e.activation(out=out, in_=in_, func=func, **kw), reads=reads, writes=writes)

    def tt(eng, out, in0, in1, op, reads, writes):
        return A(eng, lambda e: e.tensor_tensor(out=out, in0=in0, in1=in1, op=op), reads=reads, writes=writes)

    def ts(eng, out, in0, s1, s2, op0, op1, reads, writes):
        if s2 is None:
            return A(eng, lambda e: e.tensor_scalar(out=out, in0=in0, scalar1=s1, scalar2=None, op0=op0), reads=reads, writes=writes)
        return A(eng, lambda e: e.tensor_scalar(out=out, in0=in0, scalar1=s1, scalar2=s2, op0=op0, op1=op1), reads=reads, writes=writes)

    def stt(out, in0, scalar, in1, op0, op1, reads, writes, accum=None):
        if accum is None:
            return A("dve", lambda e: e.scalar_tensor_tensor(out=out, in0=in0, scalar=scalar, in1=in1, op0=op0, op1=op1), reads=reads, writes=writes)
        return A("dve", lambda e: e.scalar_tensor_tensor(out=out, in0=in0, scalar=scalar, in1=in1, op0=op0, op1=op1, accum_out=accum), reads=reads, writes=writes)

    def cp(eng, out, in_, reads, writes):
        if eng == "act":
            return A("act", lambda e: e.copy(out=out, in_=in_), reads=reads, writes=writes)
        return A(eng, lambda e: e.tensor_copy(out=out, in_=in_), reads=reads, writes=writes)

    def dma(q, out, in_, key, reads, writes, **kw):
        return A(q, lambda e: e.dma_start(out=out, in_=in_, **kw), reads=reads, writes=writes, dma=key)

    def memset(eng, ap, val, writes):
        return A(eng, lambda e: e.memset(ap, val), writes=writes)

    with st:
        for kc in range(8):
            dma("pool", wbig[:, kc * 5120:(kc + 1) * 5120], w_in_d[0, kc * 128:(kc + 1) * 128, :], "win%d" % kc, [], ["win%d" % kc])
        memset("pool", ones_f[:], 1.0, ["ones_f"])
        memset("pool", ones_b[:], 1.0, ["ones_b"])
        memset("pool", nhalf[:], -0.5, ["nhalf"])
        A("pool", lambda e: e.affine_select(out=ident_f[:], in_=ones_f[:], pattern=[[1, 128]], compare_op=ALU.is_equal,
                                            fill=0.0, base=0, channel_multiplier=-1), reads=["ones_f"], writes=["ident_f"])
        A("pool", lambda e: e.affine_select(out=ident_b[:], in_=ones_f[:], pattern=[[1, 128]], compare_op=ALU.is_equal,
                                            fill=0.0, base=0, channel_multiplier=-1), reads=["ones_f"], writes=["ident_b"])
        A("pool", lambda e: e.affine_select(out=tri_f[:], in_=ones_f[:], pattern=[[1, 128]], compare_op=ALU.is_ge,
                                            fill=0.0, base=0, channel_multiplier=-1), reads=["ones_f"], writes=["tri_f"])
        A("pool", lambda e: e.affine_select(out=stri_b[:], in_=ones_f[:], pattern=[[1, 128]], compare_op=ALU.is_gt,
                                            fill=0.0, base=0, channel_multiplier=-1), reads=["ones_f"], writes=["stri_b"])
        for h in range(4):
            cp("pool", hmask4[:, h * 128:(h + 1) * 128], tri_f[:], ["tri_f"], ["hmask4"])
        for h in range(4):
            memset("pool", hmask4[0:64, h * 128 + 64:(h + 1) * 128], 0.0, ["hmask4"])
        memset("pool", rmask[:], 1.0, ["rmask"])
        for c in range(0, 512, 64):
            memset("pool", rmask[:, c:c + 1], 0.0, ["rmask"])
        A("pool", lambda e: e.iota(out=bstart[:], pattern=[[0, 1]], base=0, channel_multiplier=BR,
                                   allow_small_or_imprecise_dtypes=True), writes=["bstart"])
        memset("pool", runtot[:], 0.0, ["runtot"])
        memset("pool", pmask[:], 1.0, ["pmask"])
        memset("pool", blkrow[:], 0.0, ["blkrow"])
        memset("pool", rt[:], 0.0, ["rt"])
        memset("pool", rt[:, 44:48], -1.0e30, ["rt"])
        memset("pool", ridx[:], 0, ["ridx"])
        A("pool", lambda e: e.iota(out=io8[:], pattern=[[1, 8]], base=0, channel_multiplier=0,
                                   allow_small_or_imprecise_dtypes=True), writes=["io8"])
        memset("pool", sm[:], 0.0, ["sm0", "sm1", "sm2", "sm3", "sm4", "sm6", "sm8"])
        memset("pool", pb[:], 0.0, ["pb0", "pb1", "pb2", "pb3", "pb4", "pb5"])
        memset("pool", dec[:], 0.0, ["dec0", "dec1"])
        memset("pool", pmask[0:1, :], 0.0, ["pmask"])
        A("pool", lambda e: e.iota(out=pid[:], pattern=[[0, 1]], base=0, channel_multiplier=1,
                                   allow_small_or_imprecise_dtypes=True), writes=["pid"])
        A("pool", lambda e: e.iota(out=Jt[:], pattern=[[BR, 96]], base=0, channel_multiplier=0,
                                   allow_small_or_imprecise_dtypes=True), writes=["Jt"])

        memset("pool", bigf2[:], 0.0, ["bigf2"])
        zsrc = bigf2[:].bitcast(BF16).rearrange("p (r d) -> p r d", r=2)
        zero_ops = []
        for zi in range(2):
            dma("sp", zsrc_d[zi * 256:(zi + 1) * 256, :].rearrange("(r p) d -> p r d", p=128), zsrc, "zsrc", ["bigf2"], ["zsrc_d"])
        WIN = ["win%d" % k for k in range(8)]
        dma("pool", wua[:], w_up_a_d[0].rearrange("(c p) n -> p c n", p=128), "wua", [], ["wua"])
        dma("pool", wub[:], w_up_b_d[0].rearrange("(c p) n -> p c n", p=128), "wub", [], ["wub"])
        dma("pool", wo[:], w_out_d[0].rearrange("(c p) n -> p c n", p=128), "wo", [], ["wo"])
        dma("pool", wr[:, :, 0:4], w_grp_d[0].rearrange("(c p) n -> p c n", p=128), "wr0", [], ["wr0"])
        dma("pool", wr[:, :, 4:36], w_exp_d[0].rearrange("(c p) n -> p c n", p=128), "wr1", [], ["wr1"])
        dma("pool", gbc0[:], g_mix_d.partition_broadcast(128), "gbc0", [], ["gbc0"])
        dma("pool", lng_bc[:], ln_g_d.partition_broadcast(128), "lng", [], ["lng_bc"])
        dma("pool", lnb_bc[:], ln_b_d.partition_broadcast(128), "lnb", [], ["lnb_bc"])
        dma("pool", ng_bc[:], ng_d.partition_broadcast(128), "ngb", [], ["ng_bc"])
        dma("pool", gbc1[:], g_ffn_d.partition_broadcast(128), "gbc1", [], ["gbc1"])
        dma("sp", bsp[:], b_sp_d[0].rearrange("g t -> t g"), "bsp", [], ["bsp"], allow_slow_non_contiguous=True)
        dma("sp", lbp[:], lbp_d.rearrange("s (h k) -> k s h", k=128), "lbp", [], ["lbp"], allow_slow_non_contiguous=True)
        tt("dve", lb[:], lbp[:, 0, :], lbp[:, 1, :], ALU.subtract, ["lbp"], ["lb"])
        act(lb[:], lb[:], AF.Sigmoid, ["lb"], ["lb"])
        ts("dve", oml[:], lb[:], -1.0, 1.0, ALU.mult, ALU.add, ["lb"], ["oml"])
        wraw = bigf2
        dma("sp", wraw[:].rearrange("p (g s) -> p g s", g=8), w_sp_d[0].rearrange("g t s -> t g s"), "wraw", [], ["bigf2"])
        for half in range(2):
            for j in range(4):
                g = half * 4 + j
                tr(pQ[:, j * 128:(j + 1) * 128], wraw[:, g * 128:(g + 1) * 128], ident_f[:], ["bigf2", "ident_f"], ["pQ"])
            for j in range(4):
                g = half * 4 + j
                tt("dve", wspT[:, g, :], pQ[:, j * 128:(j + 1) * 128], tri_f[:], ALU.mult, ["pQ", "tri_f"], ["wspT"])

        def rmsnorm_to_bf16(src, src_key, gbc, gkey, dst, dst_key, smcol):
            act(dst[:], src[:], AF.Square, [src_key], [dst_key, "sm%d" % smcol], accum_out=sm[:, smcol:smcol + 1])
            ts("pool", sm[:, smcol:smcol + 1], sm[:, smcol:smcol + 1], 1.0 / D, EPS, ALU.mult, ALU.add, ["sm%d" % smcol], ["sm%d" % smcol])
            tt("pool", sm[:, smcol:smcol + 1], sm[:, smcol:smcol + 1], nhalf[:, 0:1], ALU.pow, ["sm%d" % smcol, "nhalf"], ["sm%d" % smcol])
            stt(dst[:], src[:], sm[:, smcol:smcol + 1], gbc[:], ALU.mult, ALU.mult, [src_key, "sm%d" % smcol, gkey], [dst_key])

        def transpose8(src, src_key, dst, dst_key, n=8, stride=None):
            for kc in range(n):
                if stride is None:
                    sl = src[:, kc * 128:(kc + 1) * 128]
                else:
                    sl = src[:, kc:128 * stride:stride]
                tr(pT[:, kc * 128:(kc + 1) * 128], sl, ident_b[:], [src_key, "ident_b"], ["pT"])
            cp("act", dst[:, 0:n, :].rearrange("p c t -> p (c t)") if False else dst[:].rearrange("p c t -> p (c t)"), pT[:, 0:n * 128], ["pT"], [dst_key])

        def gelu_from_psum(pbank, pkey, dst, dst_key):
            act(tmpA[:], pbank[:], AF.Square, [pkey], ["tmpA"], scale=0.21145921592579985)
            stt(tmpA[:], tmpA[:], 1.0, pbank[:], ALU.add, ALU.mult, ["tmpA", pkey], ["tmpA"])
            act(tmpA[:], tmpA[:], AF.Sigmoid, ["tmpA"], ["tmpA"], scale=1.5957691216057308)
            tt("dve", dst[:], tmpA[:], pbank[:], ALU.mult, ["tmpA", pkey], [dst_key])

        last_inproj = [None]

        def front(t):
            par = t % 2
            xb_ = xt[par]
            xk = "xt%d" % par
            dma("sp", xb_[:], x_d[t * 128:(t + 1) * 128, :], xk, [], [xk])
            yield
            rmsnorm_to_bf16(xb_, xk, gbc0, "gbc0", B8b, "B8b", 0)
            yield
            yield
            transpose8(B8b, "B8b", T8f, "T8f")
            hT = T8f
            yield
            for (pb_, pk, c0) in ((pQ, "pQ", 1024), (pF, "pF", 1536)):
                for h in range(4):
                    for kc in range(8):
                        last_inproj[0] = mm(pb_[:, h * 128:(h + 1) * 128], wbig[:, kc * 5120 + c0 + h * 128: kc * 5120 + c0 + (h + 1) * 128],
                                            hT[:, kc, :], kc == 0, kc == 7, ["T8f"] + WIN, [pk])
                if pk == "pQ":
                    act(F6[0][:], pQ[:], AF.Sigmoid, ["pQ"], ["F6_0"])
                    tt("dve", F6[0][:], F6[0][:], pQ[:], ALU.mult, ["F6_0", "pQ"], ["F6_0"])
                else:
                    act(F1[0][:], pF[:], AF.Sigmoid, ["pF"], ["F1_0"])
                yield

            def inproj_tok(c0):
                bank, bk = nextM()
                for kc in range(8):
                    last_inproj[0] = mm(bank[:], hT[:, kc, :], wbig[:, kc * 5120 + c0: kc * 5120 + c0 + 512], kc == 0, kc == 7, ["T8f"] + WIN, [bk])
                return bank, bk

            bv, bvk = inproj_tok(512)
            gelu_from_psum(bv, bvk, gv, "gv")
            yield
            F1p, F1k = F1[0], "F1_0"
            for h in range(4):
                ts("dve", F1p[:, h * 128:(h + 1) * 128], F1p[:, h * 128:(h + 1) * 128], oml[:, h:h + 1], lb[:, h:h + 1], ALU.mult, ALU.add,
                   [F1k, "oml", "lb"], [F1k])
            ts("pool", F2[:], F1p[:], -1.0, 1.0, ALU.mult, ALU.add, [F1k], ["F2"])
            tt("pool", F3[:], F1p[:], rmask[:], ALU.mult, [F1k, "rmask"], ["F3"])
            tt("pool", F4[:], F1p[:], F3[:], ALU.subtract, [F1k, "F3"], ["F4"])
            yield
            A("dve", lambda e: e.bn_stats(out=st6[:], in_=gv[:]), reads=["gv"], writes=["st6"])
            A("dve", lambda e: e.bn_aggr(out=sm[:, 4:6], in_=st6[:]), reads=["st6"], writes=["sm4"])
            ts("pool", sm[:, 6:7], sm[:, 5:6], EPS, None, ALU.add, None, ["sm4"], ["sm6"])
            tt("pool", sm[:, 6:7], sm[:, 6:7], nhalf[:, 0:1], ALU.pow, ["sm6", "nhalf"], ["sm6"])
            ts("dve", gv[:], gv[:], sm[:, 4:5], sm[:, 6:7], ALU.subtract, ALU.mult, ["gv", "sm4", "sm6"], ["gv"])
            tt("pool", gv[:], gv[:], lng_bc[:], ALU.mult, ["gv", "lng_bc"], ["gv"])
            tt("pool", vn[par][:], gv[:], lnb_bc[:], ALU.add, ["gv", "lnb_bc"], ["vn%d" % par])
            yield
            bu, buk = inproj_tok(0)
            gelu_from_psum(bu, buk, gu[par], "gu%d" % par)
            yield
            A("dve", lambda e: e.tensor_tensor_scan(out=F5[:], data0=F3[:], data1=F4[:], initial=0.0, op0=ALU.mult, op1=ALU.add),
              reads=["F3", "F4"], writes=["F5"])
            A("dve", lambda e: e.reciprocal(out=F3[:], in_=F5[:]), reads=["F5"], writes=["F3"])
            tt("dve", qdT[par][:], F6[0][:], F5[:], ALU.mult, ["F6_0", "F5"], ["qdT%d" % par])
            tt("pool", kiT[par][:], F2[:], F3[:], ALU.mult, ["F2", "F3"], ["kiT%d" % par])
            yield
            bi, bik = inproj_tok(2048)
            cp("act", v_tok[par][:], bi[:], [bik], ["v_tok%d" % par])
            yield
            for h in range(4):
                for c in range(2):
                    lo = h * 128 + c * 64
                    stt(keT[par][:, lo:lo + 64], F2[:, lo:lo + 64], F5[:, lo + 63:lo + 64], F3[:, lo:lo + 64], ALU.mult, ALU.mult,
                        ["F2", "F5", "F3"], ["keT%d" % par])
            cp("pool", dec[:, par * 8:(par + 1) * 8], F5[:, 63:512:64], ["F5"], ["dec%d" % par])
            yield
            bo, bok = inproj_tok(2560)
            act(sog[par][:], bo[:], AF.Sigmoid, [bok], ["sog%d" % par])
            yield
            for j in range(2):
                bg, bgk = inproj_tok(3072 + j * 512)
                act(sga[par][:, j * 512:(j + 1) * 512], bg[:], AF.Sigmoid, [bgk], ["sga%d" % par])
                yield
            for j in range(2):
                bg, bgk = inproj_tok(4096 + j * 512)
                act(sgb[par][:, j * 512:(j + 1) * 512], bg[:], AF.Sigmoid, [bgk], ["sgb%d" % par])
                yield

        gdone = {}

        def back_g(t):
            par = t % 2
            bs_, bsk = nextM()
            for g in range(8):
                gs = slice(g * 64, (g + 1) * 64)
                mm(bs_[:, gs], wspT[:, g, :], vn[par][:, gs], True, True, ["wspT", "vn%d" % par], [bsk])
            for g in range(8):
                gs = slice(g * 64, (g + 1) * 64)
                stt(ya[:, gs], bs_[:, gs], bsp[:, g:g + 1], gu[par][:, gs], ALU.add, ALU.mult, [bsk, "bsp", "gu%d" % par], ["ya"])
            yield
            T4g = T8[:, 4:8, :]
            transpose8(ya, "ya", T4g, "T8", n=4)
            yield
            banksA = []
            for half in range(2):
                bk_, bkk = nextM()
                for c in range(4):
                    mm(bk_[:], T4g[:, c, :], wua[:, c, half * 512:(half + 1) * 512], c == 0, c == 3, ["T8", "wua"], [bkk])
                banksA.append((bk_, bkk))
            for half in range(2):
                bk_, bkk = banksA[half]
                tt("dve", bigf2[:, half * 512:(half + 1) * 512], bk_[:], sga[par][:, half * 512:(half + 1) * 512], ALU.mult, [bkk, "sga%d" % par], ["bigf2"])
            yield
            gdone[t] = True
            yield

        def back(t):
            par = t % 2
            xb_ = xt[par]
            xk = "xt%d" % par
            qd, qdk = qdT[par], "qdT%d" % par
            ki, kik = kiT[par], "kiT%d" % par
            ke, kek = keT[par], "keT%d" % par
            vtk, vtkk = v_tok[par], "v_tok%d" % par
            if t % TPS == 0:
                memset("pool", Sst[:], 0.0, ["Sst"])
                memset("pool", Sbf[:], 0.0, ["Sbf"])
            for h in range(4):
                tr(pT[:, h * 128:(h + 1) * 128], ke[:, h * 128:(h + 1) * 128], ident_b[:], [kek, "ident_b"], ["pT"])
            cp("act", ke_tok[:], pT[:, 0:512], ["pT"], ["ke_tok"])
            for h in range(4):
                hs = slice(h * 128, (h + 1) * 128)
                mm(pA[:, hs], ki[:, hs], qd[:, hs], True, True, [kik, qdk], ["pA"])
            tt("dve", attT[:], pA[:], hmask4[:], ALU.mult, ["pA", "hmask4"], ["attT"])
            yield
            if t >= 1:
                rank_step(t - 1)
            for h in range(4):
                hs = slice(h * 128, (h + 1) * 128)
                mm(pD[:, hs], ke_tok[0:64, hs], vtk[0:64, hs], True, True, ["ke_tok", vtkk], ["pD"])
            for h in range(4):
                hs = slice(h * 128, (h + 1) * 128)
                mm(pO[:, hs], attT[:, hs], vtk[:, hs], h == 0, False, ["attT", vtkk], ["pO"], skip_group_check=True)
                mm(pO[0:64, hs], qd[:, h * 128:h * 128 + 64], Sbf[:, hs], False, False, [qdk, "Sbf"], ["pO"], skip_group_check=True)
            for h in range(4):
                hs = slice(h * 128, (h + 1) * 128)
                stt(Sst[:, hs], Sst[:, hs], dec[:, par * 8 + 2 * h:par * 8 + 2 * h + 1], pD[:, hs], ALU.mult, ALU.add, ["Sst", "dec%d" % par, "pD"], ["Sst"])
            cp("pool", Sbf[:], Sst[:], ["Sst"], ["Sbf"])
            yield
            for h in range(4):
                hs = slice(h * 128, (h + 1) * 128)
                mm(pD[:, hs], ke_tok[64:128, hs], vtk[64:128, hs], True, True, ["ke_tok", vtkk], ["pD"])
            for h in range(4):
                hs = slice(h * 128, (h + 1) * 128)
                mm(pO[64:128, hs], qd[:, h * 128 + 64:h * 128 + 128], Sbf[:, hs], False, True, [qdk, "Sbf"], ["pO"], skip_group_check=True)
            for h in range(4):
                hs = slice(h * 128, (h + 1) * 128)
                stt(Sst[:, hs], Sst[:, hs], dec[:, par * 8 + 2 * h + 1:par * 8 + 2 * h + 2], pD[:, hs], ALU.mult, ALU.add, ["Sst", "dec%d" % par, "pD"], ["Sst"])
            cp("pool", Sbf[:], Sst[:], ["Sst"], ["Sbf"])
            yield
            for h in range(4):
                hs = slice(h * 128, (h + 1) * 128)
                act(tmpB[:, hs], pO[:, hs], AF.Square, ["pO"], ["m2h", "sm8"], accum_out=sm[:, 8 + h:9 + h])
            ts("pool", sm[:, 8:12], sm[:, 8:12], 1.0 / 128, EPS, ALU.mult, ALU.add, ["sm8"], ["sm8"])
            tt("pool", sm[:, 8:12], sm[:, 8:12], nhalf[:, 0:4], ALU.pow, ["sm8", "nhalf"], ["sm8"])
            for h in range(4):
                hs = slice(h * 128, (h + 1) * 128)
                stt(tmpB[:, hs], pO[:, hs], sm[:, 8 + h:9 + h], ng_bc[:, hs], ALU.mult, ALU.mult, ["pO", "sm8", "ng_bc"], ["m2h"])
            tt("pool", yb[:], tmpB[:], sog[par][:], ALU.mult, ["m2h", "sog%d" % par], ["yb"])
            yield
            yield
            assert gdone.get(t)
            transpose8(yb, "yb", T4, "T4", n=4)
            yield
            for half in range(2):
                hs = slice(half * 512, (half + 1) * 512)
                bk_, bkk = nextM()
                for c in range(4):
                    mm(bk_[:], T4[:, c, :], wub[:, c, hs], c == 0, c == 3, ["T4", "wub"], [bkk])
                tt("dve", m2h[:], bk_[:], sgb[par][:, hs], ALU.mult, [bkk, "sgb%d" % par], ["m2h"])
                tt("pool", B8[:, hs], m2h[:], bigf2[:, hs], ALU.add, ["m2h", "bigf2"], ["B8"])
            yield
            yield
            transpose8(B8, "B8", T8, "T8")
            yield
            for half in range(2):
                hs = slice(half * 512, (half + 1) * 512)
                bk_, bkk = nextM()
                for kc in range(8):
                    mm(bk_[:], T8[:, kc, :], wo[:, kc, hs], kc == 0, kc == 7, ["T8", "wo"], [bkk])
                tt("dve", xb_[:, hs], bk_[:], xb_[:, hs], ALU.add, [bkk, xk], [xk])
            dma("sp", x1_d[t * 128:(t + 1) * 128, :], xb_[:], "x1st%d" % (t % 2), [xk], ["x1_d"])
            if dbg:
                dma("sp", dbg_d["x1"][t * 128:(t + 1) * 128, :], xb_[:], "dbgx1", [xk], [])
            yield
            rmsnorm_to_bf16(xb_, xk, gbc1, "gbc1", B8, "B8", 1)
            dma("sp", h2_d[t * 128:(t + 1) * 128, :], B8[:], "h2st", ["B8"], ["h2_d"])
            yield
            yield
            transpose8(B8, "B8", T8, "T8")
            yield
            for kc in range(8):
                mm(pA[:, 0:36], T8[:, kc, :], wr[:, kc, :], kc == 0, kc == 7, ["T8", "wr0", "wr1"], ["pA"])
            cp("dve", lg[:], pA[:, 0:36], ["pA"], ["lg"])
            yield
            cp("pool", gla[:, t, :], lg[:, 0:4], ["lg"], ["gla"])
            cp("dve", rt[:, 40:44], lg[:, 0:4], ["lg"], ["rt"])
            A("dve", lambda e: e.max(out=rt[:, 48:56], in_=rt[:, 40:48]), reads=["rt"], writes=["rt"])
            A("dve", lambda e: e.max_index(out=ridx[:, 0:8], in_max=rt[:, 48:56], in_values=rt[:, 40:48]), reads=["rt"], writes=["ridx"])
            cp("dve", gmxa[:, t:t + 1], rt[:, 48:49], ["rt"], ["gmxa"])
            cp("dve", rt[:, 56:57], ridx[:, 0:1], ["ridx"], ["rt"])
            ts("dve", rt[:, 0:4], io8[:, 0:4], rt[:, 56:57], None, ALU.is_equal, None, ["io8", "rt"], ["rt"])
            ts("dve", rt[:, 8:16], lg[:, 4:12], rt[:, 0:1], None, ALU.mult, None, ["lg", "rt"], ["rt"])
            for g in range(1, 4):
                stt(rt[:, 8:16], lg[:, 4 + g * 8:12 + g * 8], rt[:, g:g + 1], rt[:, 8:16], ALU.mult, ALU.add, ["lg", "rt"], ["rt"])
            A("dve", lambda e: e.max(out=rt[:, 16:24], in_=rt[:, 8:16]), reads=["rt"], writes=["rt"])
            A("dve", lambda e: e.max_index(out=ridx[:, 8:16], in_max=rt[:, 16:24], in_values=rt[:, 8:16]), reads=["rt"], writes=["ridx"])
            tt("dve", d12a[:, t:t + 1], rt[:, 16:17], rt[:, 17:18], ALU.subtract, ["rt"], ["d12a"])
            cp("dve", rt[:, 58:60], ridx[:, 8:10], ["ridx"], ["rt"])
            ts("dve", rt[:, 24:32], io8[:], rt[:, 58:59], None, ALU.is_equal, None, ["io8", "rt"], ["rt"])
            ts("dve", rt[:, 32:40], io8[:], rt[:, 59:60], None, ALU.is_equal, None, ["io8", "rt"], ["rt"])
            for g in range(4):
                ts("dve", M1a[:, t, g * 8:(g + 1) * 8], rt[:, 24:32], rt[:, g:g + 1], None, ALU.mult, None, ["rt"], ["M1a"])
                ts("pool", M2a[:, t, g * 8:(g + 1) * 8], rt[:, 32:40], rt[:, g:g + 1], None, ALU.mult, None, ["rt"], ["M2a"])
            tt("pool", Msum[par][:], M1a[:, t, :], M2a[:, t, :], ALU.add, ["M1a", "M2a"], ["Msum%d" % par])
            yield

        def rank_step(t):
            par = t % 2
            mm(pA[:, 64:96], stri_b[:], Msum[par][:], True, True, ["stri_b", "Msum%d" % par], ["pA"])
            mm(pA[:, 96:128], ones_b[:], Msum[par][:], True, True, ["ones_b", "Msum%d" % par], ["pA"])
            tt("dve", rka[:, t, :], pA[:, 64:96], runtot[:], ALU.add, ["pA", "runtot"], ["rka"])
            tt("dve", runtot[:], pA[:, 96:128], runtot[:], ALU.add, ["pA", "runtot"], ["runtot"])

        def interleave(gens):
            gens = [g for g in gens if g is not None]
            while gens:
                for g in list(gens):
                    try:
                        next(g)
                    except StopIteration:
                        gens.remove(g)

        if "A" in phases:
            interleave([front(0)])
            nz = NBLK
            zdone = 0
            for t in range(nt):
                for _ in range(2):
                    if zdone < nz and t >= 1:
                        zero_ops.append(dma("sp", xbuf_d[zdone * BR:(zdone + 1) * BR, :], zsrc_d[0:BR, :], "xzero", ["zsrc_d"], []))
                        zdone += 1
                fr_ = front(t + 1) if t + 1 < nt else None
                if KV == 1:
                    interleave([back(t), fr_, back_g(t)])
                elif KV == 2:
                    interleave([fr_, back(t), back_g(t)])
                else:
                    interleave([fr_, back_g(t), back(t)])
                if MODE == 1 and t + 1 < nt:
                    interleave([front(t + 1)])

        if "A" in phases:
            rank_step(nt - 1)
            while zdone < nz:
                zero_ops.append(dma("sp", xbuf_d[zdone * BR:(zdone + 1) * BR, :], zsrc_d[0:BR, :], "xzero", ["zsrc_d"], []))
                zdone += 1
        E1 = [0, 12288, 24576]
        OFF_PG = 0
        OFF_PLE = 8192
        if "B" in phases:
            lastA = [last_inproj[0]]
            BF3 = ["bigf3", "qdT1", "kiT1", "gv"]
            dma("pool", gbc0[:], g_ple_d.partition_broadcast(128), "gbc0", [], ["gbc0"])
            dma("sp", F1[0][:], g_final_d[:, 0:512].partition_broadcast(128), "gfin0", [], ["F1_0"])
            dma("sp", F2[:], g_final_d[:, 512:1024].partition_broadcast(128), "gfin1", [], ["F2"])
            tot = pb[:, 0, :]
            pad = pb[:, 1, :]
            pend = pb[:, 2, :]
            pstart = pb[:, 3, :]
            tmp32 = pb[:, 4, :]
            ones32 = pb[:, 5, :]
            memset("dve", ones32, 1.0, ["pb5"])
            cmp3 = bigf3[:].rearrange("p (e j) -> p e j", e=32)
            tt("dve", cmp3, runtot[:, :, None].to_broadcast([128, 32, 32]), Jt[:, None, 0:32].to_broadcast([128, 32, 32]),
               ALU.is_gt, ["runtot", "Jt"], BF3)
            A("dve", lambda e: e.tensor_reduce(out=pad, in_=cmp3, axis=AX.X, op=ALU.add), reads=["bigf3"], writes=["pb1"])
            ts("dve", pad, pad, float(BR), None, ALU.mult, None, ["pb1"], ["pb1"])
            A("dve", lambda e: e.tensor_tensor_scan(out=pend, data0=ones32, data1=pad, initial=0.0, op0=ALU.mult, op1=ALU.add),
              reads=["pb5", "pb1"], writes=["pb2"])
            tt("dve", pstart, pend, pad, ALU.subtract, ["pb2", "pb1"], ["pb3"])
            for bp in range(2):
                tt("dve", cmp3, pend[:, None, :].to_broadcast([128, 32, 32]), Jt[:, bp * 32:(bp + 1) * 32, None].to_broadcast([128, 32, 32]),
                   ALU.is_le, ["pb2", "Jt"], BF3)
                A("dve", lambda e, bp=bp: e.tensor_reduce(out=blkrow[:, bp * 32:(bp + 1) * 32], in_=cmp3, axis=AX.X, op=ALU.add),
                  reads=["bigf3"], writes=["blkrow"])
            ts("dve", blkrow[:], blkrow[:], 31.0, 128.0, ALU.min, ALU.mult, ["blkrow"], ["blkrow"])
            ts("dve", blkrow[:], blkrow[:], pid[:, 0:1], None, ALU.add, None, ["blkrow", "pid"], ["blkrow"])
            unu = bigf2[:, 0:96]
            ts("dve", unu, Jt[:, 0:96], pb[:, 2, 31:32], None, ALU.is_ge, None, ["Jt", "pb2"], ["bigf2"])
            ts("dve", unu, unu, pmask[:, 0:1], 4096.0, ALU.mult, ALU.mult, ["bigf2", "pmask"], ["bigf2"])
            tt("dve", blkrow[:, SKIP0:64], blkrow[:, SKIP0:64], unu[:, SKIP0:64], ALU.add, ["blkrow", "bigf2"], ["blkrow"])
            cp("dve", idxall[:], blkrow[:], ["blkrow"], ["idxall"])
            ts("dve", tmp32, pend, bstart[:, 0:1], None, ALU.is_le, None, ["pb2", "bstart"], ["pb4"])
            A("dve", lambda e: e.tensor_reduce(out=metaf[:, 0:1], in_=tmp32, axis=AX.X, op=ALU.add), reads=["pb4"], writes=["metaf"])
            ts("dve", metaf[:, 0:1], metaf[:, 0:1], 31.0, None, ALU.min, None, ["metaf"], ["metaf"])
            ts("dve", metaf[:, 1:2], pb[:, 2, 31:32], bstart[:, 0:1], None, ALU.is_gt, None, ["pb2", "bstart"], ["metaf"])
            cp("dve", metai[:], metaf[:], ["metaf"], ["metai"])
            dma("sp", meta_d, metai[:], "meta", ["metai"], ["meta_d"])
            if dbg:
                dma("sp", dbg_d["meta"], metai[:], "dbgm", ["metai"], [])
            for t in range(nt):
                tt("dve", tmp32, rka[:, t, :], pstart, ALU.add, ["rka", "pb3"], ["pb4"])
                stt(pb[:, 5, :], M1a[:, t, :], 1.0, tmp32, ALU.mult, ALU.mult, ["M1a", "pb4"], ["pb5", "d1f"], accum=d1f[:, t:t + 1])
                stt(pb[:, 5, :], M2a[:, t, :], 1.0, tmp32, ALU.mult, ALU.mult, ["M2a", "pb4"], ["pb5", "d2f"], accum=d2f[:, t:t + 1])
            cp("dve", dsti[:, 0:nt], d1f[:, 0:nt], ["d1f"], ["dsti"])
            cp("dve", dsti[:, NT:NT + nt], d2f[:, 0:nt], ["d2f"], ["dsti"])
            if dbg:
                dma("sp", dbg_d["dest"], dsti[:], "dbgd", ["dsti"], [])
            tt("dve", gla[:], gla[:], gmxa[:, :, None].to_broadcast([128, NT, 4]), ALU.subtract, ["gla", "gmxa"], ["gla"])
            act(gla[:], gla[:], AF.Exp, ["gla"], ["gla"])
            A("dve", lambda e: e.tensor_reduce(out=gmxa[:], in_=gla[:], axis=AX.X, op=ALU.add), reads=["gla"], writes=["gmxa"])
            A("dve", lambda e: e.reciprocal(out=gmxa[:], in_=gmxa[:]), reads=["gmxa"], writes=["gmxa"])
            act(d12a[:], d12a[:], AF.Sigmoid, ["d12a"], ["d12a"])
            tt("dve", g1a[:], d12a[:], gmxa[:], ALU.mult, ["d12a", "gmxa"], ["g1a"])
            tt("dve", g2a[:], gmxa[:], g1a[:], ALU.subtract, ["gmxa", "g1a"], ["g2a"])
            if dbg:
                dma("sp", dbg_d["gate"][:, 0:NT], g1a[:], "dbgg", ["g1a"], [])
                dma("sp", dbg_d["gate"][:, NT:2 * NT], g2a[:], "dbgg", ["g2a"], [])
            def emit_wload(b):
                j = b % 3
                base = E1[j]

                def wload(e, b=b, base=base):
                    off = bass.IndirectOffsetOnAxis(ap=idxall[:, b:b + 1], axis=0)
                    kw = dict(bounds_check=4095, oob_is_err=False) if b >= SKIP0 else {}
                    i0 = e.indirect_dma_start(out=wbig[:, base:base + 4096], out_offset=None,
                                              in_=w1_d[0].rearrange("e (p j) n -> (e p) (j n)", j=8), in_offset=off, **kw)
                    i1 = e.indirect_dma_start(out=wbig[:, base + 4096:base + 8192], out_offset=None,
                                              in_=w3_d[0].rearrange("e (p j) n -> (e p) (j n)", j=8), in_offset=off, **kw)
                    i2 = e.indirect_dma_start(out=wbig[:, base + 8192:base + 12288], out_offset=None,
                                              in_=w2_d[0].rearrange("e (p j) n -> (e p) (j n)", j=4), in_offset=off, **kw)
                    return [i0, i1, i2]
                op = A("pool", wload, reads=["idxall"], writes=["we_%d" % j], dma="we_%d" % j, n=3)
                if b < 3 and last_inproj[0] is not None:
                    op.deps.add(last_inproj[0]); last_inproj[0].has_dep = True
            for b0 in range(min(3, nblk)):
                emit_wload(b0)
            for t in range(nt):
                hb = B8 if t % 2 == 0 else B8b
                hk = "B8" if t % 2 == 0 else "B8b"
                dma("sp", hb[:], h2_d[t * 128:(t + 1) * 128, :], "h2ld%d" % (t % 2), ["h2_d"], [hk])
                for k in range(2):
                    col = k * NT + t
                    A("pool", lambda e, col=col, hb=hb: e.indirect_dma_start(
                        out=xbuf_d, out_offset=bass.IndirectOffsetOnAxis(ap=dsti[:, col:col + 1], axis=0), in_=hb[:], in_offset=None),
                      reads=[hk, "dsti"], writes=["xbuf_w%d_%d" % (k, t % 2)], dma="scat%d_%d" % (k, t % 2))
                    if t == 0:
                        S.ops[-1].deps.update(zero_ops)

        if "C" in phases:
            scat_ops = [op for op in S.ops if op.dma is not None and op.dma.startswith("scat")]
            NSB = BR // 128
            ABK = [((pM[0], "pM0"), (pM[1], "pM1")), ((pQ, "pQ"), (pF, "pF"))]

            XB3 = [(B8, "B8"), (B8b, "B8b"), (sga[0], "sga0")]

            def c0(q):
                b, sbi = divmod(q, NSB)
                r0 = b * BR + sbi * 128
                xb_sb, xbk = XB3[q % 3]
                op = dma("sp", xb_sb[:], xbuf_d[r0:r0 + 128, :], "xbld%d" % (q % 3), [], [xbk])
                if q < 3:
                    for so in scat_ops:
                        op.deps.add(so)

            def c1(q):
                b, sbi = divmod(q, NSB)
                j = b % 3
                base = E1[j]
                qp = q % 2
                r0 = b * BR + sbi * 128
                xb_sb, xbk = XB3[q % 3]
                T8q, T8qk = (T8, "T8") if qp == 0 else (T8f, "T8f")
                transpose8(xb_sb, xbk, T8q, T8qk, stride=8)
                (bA, bAk), (bB, bBk) = ABK[qp]
                for kc in range(8):
                    mm(bA[:], T8q[:, kc, :], wbig[:, base + kc * 512: base + (kc + 1) * 512], kc == 0, kc == 7, [T8qk, "we_%d" % j], [bAk])
                for kc in range(8):
                    mm(bB[:], T8q[:, kc, :], wbig[:, base + 4096 + kc * 512: base + 4096 + (kc + 1) * 512], kc == 0, kc == 7, [T8qk, "we_%d" % j], [bBk])

            def c2(q):
                b, sbi = divmod(q, NSB)
                j = b % 3
                base = E1[j]
                qp = q % 2
                r0 = b * BR + sbi * 128
                (bA, bAk), (bB, bBk) = ABK[qp]
                act(tmpA[:], bA[:], AF.Sigmoid, [bAk], ["tmpA"])
                tt("dve", tmpA[:], tmpA[:], bA[:], ALU.mult, ["tmpA", bAk], ["tmpA"])
                yq, yqk = (ya, "ya") if qp == 0 else (yb, "yb")
                tt("dve", yq[:], tmpA[:], bB[:], ALU.mult, ["tmpA", bBk], [yqk])
                transpose8(yq, yqk, T4, "T4", n=4, stride=4)
                ysb = xt[qp][:, 0:512].bitcast(BF16)
                yk = "xt%d" % qp
                for half, (bY, bYk) in enumerate(((pO, "pO"), (pD, "pD"))):
                    for c in range(4):
                        mm(bY[:], T4[:, c, :], wbig[:, base + 8192 + c * 1024 + half * 512: base + 8192 + c * 1024 + (half + 1) * 512],
                           c == 0, c == 3, ["T4", "we_%d" % j], [bYk])
                    cp("act" if half == 0 else "dve", ysb[:, half * 512:(half + 1) * 512], bY[:], [bYk], [yk])
                dma("sp", ybuf_d[r0:r0 + 128, :], ysb[:], "yst%d" % qp, [yk], ["ybuf_d"])

            nq = nblk * NSB
            c0(0)
            c0(1)
            c1(0)
            for q in range(nq):
                if q + 2 < nq:
                    c0(q + 2)
                if q + 1 < nq:
                    c1(q + 1)
                c2(q)
                if (q + 1) % NSB == 0:
                    nb_ = (q + 1) // NSB + 2
                    if nb_ < nblk:
                        emit_wload(nb_)

        if "D" in phases:
            dma("pool", wbig[:, OFF_PG:OFF_PG + 8192].rearrange("p (c n) -> p c n", c=8), w_pg_d[0].rearrange("(c p) n -> p c n", p=128),
                "wpg", [], ["wpg", "we_0"])
            dma("pool", wbig[:, OFF_PLE:OFF_PLE + 2048].rearrange("p (c n) -> p c n", c=2), w_ple_d[0].rearrange("(c p) n -> p c n", p=128),
                "wple", [], ["wple", "we_0"])
            yst_ops = [op for op in S.ops if op.dma is not None and op.dma.startswith("yst")]
            last_pe = next(op for op in reversed(S.ops) if op.eng == "pe")
            last_pe.has_dep = True
            BF3 = ["bigf3", "qdT1", "kiT1", "gv"]
            T8x = [T8, T8f]
            T8k = ["T8", "T8f"]
            WF = wbig[:, 12288:40960].bitcast(F32)
            xr3 = [xt[0][:], xt[1][:], WF[:, 0:1024], WF[:, 4096:5120], WF[:, 5120:6144]]
            xr3k = ["xt0", "xt1", "wf_x2", "wf_x3", "wf_x4"]
            ygA = [bigf2[:, 0:512].bitcast(BF16), WF[:, 1024:1536].bitcast(BF16)]
            ygAk = [["bigf2"], ["wf_ya"]]
            ygB = [bigf3[:, 0:512].bitcast(BF16), WF[:, 1536:2048].bitcast(BF16)]
            ygBk = [BF3, ["wf_yb"]]
            ptl = [F3[:, 0:256], F3[:, 256:512]]
            ptlk = ["F3a", "F3b"]
            B8x = [B8, B8b]
            B8k = ["B8", "B8b"]
            ost = [(F4[:], "F4", F5[:], "F5"), (WF[:, 2048:2560], "wf_o0", WF[:, 2560:3072], "wf_o1")]
            junk = sga[0]
            B8x = [B8, B8b]
            first_wf = {}

            def wfdep(op, key):
                if key.startswith("wf_") and key not in first_wf:
                    first_wf[key] = True
                    op.deps.add(last_pe)
                return op

            def s1(t):
                i3 = t % 5
                par = t % 2
                xr, xk = xr3[i3], xr3k[i3]
                wfdep(dma("sp", xr, x1_d[t * 128:(t + 1) * 128, :], "x1ld%d" % i3, ["x1_d"], [xk]), xk)
                for k, (yb_, ybk) in enumerate(((ygA[par], ygAk[par]), (ygB[par], ygBk[par]))):
                    col = k * NT + t
                    og_ = A("pool", lambda e, col=col, yb_=yb_: e.indirect_dma_start(
                        out=yb_, out_offset=None, in_=ybuf_d, in_offset=bass.IndirectOffsetOnAxis(ap=dsti[:, col:col + 1], axis=0)),
                        reads=["dsti"], writes=ybk, dma="gath%d_%d" % (k, par))
                    wfdep(og_, ybk[0])
                    if t == 0:
                        for yo in yst_ops:
                            og_.deps.add(yo)
                dma("sp", ptl[par], p_d[t * 128:(t + 1) * 128, :], "pld%d" % par, [], [ptlk[par], "F3"])
                yield

            def s2(t):
                i3 = t % 5
                par = t % 2
                xr, xk = xr3[i3], xr3k[i3]
                stt(xr, ygA[par], g1a[:, t:t + 1], xr, ALU.mult, ALU.add, ygAk[par] + ["g1a", xk], [xk])
                stt(xr, ygB[par], g2a[:, t:t + 1], xr, ALU.mult, ALU.add, ygBk[par] + ["g2a", xk], [xk])
                if dbg:
                    dma("sp", dbg_d["x2"][t * 128:(t + 1) * 128, :], xr, "dbgx2", [xk], [])
                cp("act", ptbx[par], ptl[par], [ptlk[par]], ["ptb%d" % par])
                yield
                rmsnorm_to_bf16(xr, xk, gbc0, "gbc0", B8x[par], B8k[par], 2)
                yield

            def s2b(t):
                par = t % 2
                transpose8(B8x[par], B8k[par], T8x[par], T8k[par])
                yield
                for c in range(2):
                    tr(pT[:, c * 128:(c + 1) * 128], ptbx[par][:, c * 128:(c + 1) * 128], ident_b[:], ["ptb%d" % par, "ident_b"], ["pT"])
                cp("act", T4[:, 2 * par:2 * par + 2, :].rearrange("p c t -> p (c t)"), pT[:, 0:256], ["pT"], ["T4_%d" % par, "T4"])
                yield

            def s3(t):
                i3 = t % 5
                par = t % 2
                xr, xk = xr3[i3], xr3k[i3]
                for half in range(2):
                    hs = slice(half * 512, (half + 1) * 512)
                    bG, bGk = nextM()
                    for kc in range(8):
                        mm(bG[:], T8x[par][:, kc, :], wbig[:, OFF_PG + kc * 1024 + half * 512: OFF_PG + kc * 1024 + (half + 1) * 512],
                           kc == 0, kc == 7, [T8k[par], "wpg"], [bGk])
                    act(tmpA[:], bG[:], AF.Sigmoid, [bGk], ["tmpA"])
                    bP, bPk = (pO, "pO") if half == 0 else (pD, "pD")
                    for c in range(2):
                        mm(bP[:], T4[:, 2 * par + c, :], wbig[:, OFF_PLE + c * 1024 + half * 512: OFF_PLE + c * 1024 + (half + 1) * 512],
                           c == 0, c == 1, ["T4_%d" % par, "wple"], [bPk])
                    tt("dve", tmpA[:], tmpA[:], bP[:], ALU.mult, ["tmpA", bPk], ["tmpA"])
                    tt("dve", xr[:, hs], xr[:, hs], tmpA[:], ALU.add, [xk, "tmpA"], [xk])
                    yield

            def s3b(t):
                i3 = t % 5
                par = t % 2
                xr, xk = xr3[i3], xr3k[i3]
                act(junk[:], xr, AF.Square, [xk], ["sga0", "sm3"], accum_out=sm[:, 3:4])
                ts("pool", sm[:, 3:4], sm[:, 3:4], 1.0 / D, EPS, ALU.mult, ALU.add, ["sm3"], ["sm3"])
                tt("pool", sm[:, 3:4], sm[:, 3:4], nhalf[:, 0:1], ALU.pow, ["sm3", "nhalf"], ["sm3"])
                yield
                o0, o0k, o1, o1k = ost[par]
                wfdep(stt(o0, xr[:, 0:512], sm[:, 3:4], F1[0][:], ALU.mult, ALU.mult, [xk, "sm3", "F1_0"], [o0k]), o0k)
                wfdep(stt(o1, xr[:, 512:1024], sm[:, 3:4], F2[:], ALU.mult, ALU.mult, [xk, "sm3", "F2"], [o1k]), o1k)
                dma("sp", out_d[t * 128:(t + 1) * 128, 0:512], o0, "outst0_%d" % par, [o0k], ["out_d0"])
                dma("sp", out_d[t * 128:(t + 1) * 128, 512:1024], o1, "outst1_%d" % par, [o1k], ["out_d1"])
                yield

            ptbx = [ptb[:, 0:256], WF[:, 3072:3200].bitcast(BF16)]
            for step in range(nt + 4):
                interleave([s3b(step - 4) if 0 <= step - 4 < nt else None,
                            s3(step - 3) if 0 <= step - 3 < nt else None,
                            s2b(step - 2) if 0 <= step - 2 < nt else None,
                            s2(step - 1) if 0 <= step - 1 < nt else None,
                            s1(step) if step < nt else None])
        S.emit(nc)
    return nc


_CACHE = {}


def kernel(**inputs):
    names = ["g_mix", "w_in", "gm_ln_g", "gm_ln_b", "gm_w_sp", "gm_b_sp", "w_up_a", "hg_lb_param", "hg_norm_g", "w_up_b",
             "w_out", "g_ffn", "w_grp", "w_exp", "w1", "w3", "w2", "g_ple", "w_pg", "w_ple"]
    x = np.ascontiguousarray(np.asarray(inputs["x"], dtype=np.float32)).reshape(NCORES, TOK, D)
    p = np.ascontiguousarray(np.asarray(inputs["p"], dtype=np.float32)).reshape(NCORES, TOK, 256)
    shared = {n: np.ascontiguousarray(np.asarray(inputs[n], dtype=np.float32)) for n in names}
    shared["g_final"] = np.ascontiguousarray(np.asarray(inputs["g_final"], dtype=np.float32)).reshape(1, D)
    if "nc" not in _CACHE:
        _CACHE["nc"] = build()
    nc = _CACHE["nc"]
    in_maps = []
    for c in range(NCORES):
        m = dict(shared)
        m["x"] = x[c]
        m["p"] = p[c]
        in_maps.append(m)
    res = run_bass_kernel_spmd(nc, in_maps, core_ids=list(range(NCORES)))
    out = np.stack([np.asarray(r["out"], dtype=np.float32) for r in res.results], axis=0)
    return out.reshape(16, 2048, D)
```
